# Optimizing a Trainium2 kernel written in Bass

```python
import math
import jax
import jax.numpy as jnp
from jax import lax
import numpy as np

D_MODEL = 1024
BATCH = 16
SEQ = 2048
DEPTH = 1

N_HEADS_A = 8
HEAD_DIM_A = 64
WIDTH_A = N_HEADS_A * HEAD_DIM_A
KV_RANK = 256
N_HEADS_IDX = 8
HEAD_DIM_IDX = 64
TOPK_MAX = 256
Q_BLOCK = 128
N_BUCKETS = 32
MAX_DISTANCE = 128
N_HEADS_M = 4
HEAD_DIM_M = 128
WIDTH_M = N_HEADS_M * HEAD_DIM_M
CONV_WIDTH = 4
CHUNK = 64
N_GROUPS = 4
EXPERTS_PER_GROUP = 4
N_EXPERTS = N_GROUPS * EXPERTS_PER_GROUP
TOP_K_EXP = 2
D_EXPERT = 512
LN_EPS = 1e-5
ALPHA = (2.0 * DEPTH) ** 0.25
BETA = (8.0 * DEPTH) ** -0.25

IN_SPLITS = (
    WIDTH_A,
    KV_RANK,
    N_HEADS_IDX * HEAD_DIM_IDX,
    HEAD_DIM_IDX,
    N_HEADS_IDX,
    2 * WIDTH_M,
    WIDTH_M,
    N_HEADS_M,
    N_HEADS_M,
    WIDTH_M,
    D_MODEL,
    D_MODEL,
)
D_IN = sum(IN_SPLITS)

kernel_name = "hybrid_dsa_mlstm_hmoe_deepnorm"


def split_cols(h):
    idx = np.cumsum(IN_SPLITS)[:-1].tolist()
    return jnp.split(h, idx, axis=-1)


def layer_norm(x, g, b):
    xf = x.astype(jnp.float32)
    mu = jnp.mean(xf, axis=-1, keepdims=True)
    var = jnp.mean(jnp.square(xf - mu), axis=-1, keepdims=True)
    return ((xf - mu) * lax.rsqrt(var + LN_EPS) * g + b).astype(x.dtype)


def rms_norm(x, g):
    xf = x.astype(jnp.float32)
    return (xf * lax.rsqrt(jnp.mean(xf * xf, axis=-1, keepdims=True) + LN_EPS) * g).astype(x.dtype)


def t5_bucket(dist):
    max_exact = N_BUCKETS // 2
    d = jnp.maximum(dist, 0)
    ratio = jnp.log(jnp.maximum(d, 1).astype(jnp.float32) / max_exact) / math.log(MAX_DISTANCE / max_exact)
    large = jnp.minimum(max_exact + (ratio * (N_BUCKETS - max_exact)).astype(jnp.int32), N_BUCKETS - 1)
    return jnp.where(d < max_exact, d, large)


def causal_conv(u, w, b):
    out = lax.conv_general_dilated(
        u, w[:, None, :], window_strides=(1,), padding=[(CONV_WIDTH - 1, 0)],
        dimension_numbers=("NWC", "WIO", "NWC"), feature_group_count=u.shape[-1])
    return out + b


def dsa_attention(q, c_kv, q_idx, k_idx, w_idx, w_uk, w_uv, rel_bias):
    B, S = q.shape[0], q.shape[1]
    topk = min(TOPK_MAX, S // 4)
    n_blk = S // Q_BLOCK
    key_pos = jnp.arange(S)
    q_lat = jnp.einsum("bshd,rhd->bshr", q, w_uk) * (HEAD_DIM_A ** -0.5)

    def block(i):
        t0 = i * Q_BLOCK
        qi = lax.dynamic_slice_in_dim(q_idx, t0, Q_BLOCK, axis=1)
        wi = lax.dynamic_slice_in_dim(w_idx, t0, Q_BLOCK, axis=1)
        ql = lax.dynamic_slice_in_dim(q_lat, t0, Q_BLOCK, axis=1)
        qpos = t0 + jnp.arange(Q_BLOCK)
        dots = jnp.einsum("bthd,bsd->bths", qi, k_idx)
        score = jnp.einsum("bth,bths->bts", wi, jax.nn.relu(dots)).astype(jnp.float32)
        causal = key_pos[None, :] <= qpos[:, None]
        score = jnp.where(causal[None], score, -jnp.inf)
        _, sel = lax.top_k(score, topk)
        valid = sel <= qpos[None, :, None]
        c_sel = jax.vmap(lambda c, ix: c[ix])(c_kv, sel)
        logits = jnp.einsum("bthr,btkr->bthk", ql, c_sel).astype(jnp.float32)
        bias = rel_bias[t5_bucket(qpos[None, :, None] - sel)].astype(jnp.float32)
        logits = logits + jnp.moveaxis(bias, -1, 2)
        logits = jnp.where(valid[:, :, None, :], logits, -jnp.inf)
        p = jax.nn.softmax(logits, axis=-1).astype(c_sel.dtype)
        return jnp.einsum("bthk,btkr->bthr", p, c_sel)

    o_lat = lax.map(block, jnp.arange(n_blk))
    o_lat = jnp.moveaxis(o_lat, 0, 1).reshape(B, S, N_HEADS_A, KV_RANK)
    return jnp.einsum("bshr,rhd->bshd", o_lat, w_uv)


def mlstm(q, k, v, i_pre, f_pre):
    B, S, NH, D = q.shape
    NC = S // CHUNK
    f32 = jnp.float32

    def to_chunks(a):
        a = a.astype(f32).reshape((B, NC, CHUNK) + a.shape[2:])
        return jnp.moveaxis(a, 3, 1)

    qc, kc, vc = to_chunks(q), to_chunks(k) * (D ** -0.5), to_chunks(v)
    ic, fc = to_chunks(i_pre), to_chunks(f_pre)
    log_f = jax.nn.log_sigmoid(fc)
    bcum = jnp.cumsum(log_f, axis=-1)
    b_last = bcum[..., -1]
    causal = jnp.tril(jnp.ones((CHUNK, CHUNK), dtype=bool))
    logD = jnp.where(causal, bcum[..., :, None] - bcum[..., None, :] + ic[..., None, :], -jnp.inf)

    log_w_end = b_last[..., None] - bcum + ic
    m_loc = jnp.max(log_w_end, axis=-1)
    w_end = jnp.exp(log_w_end - m_loc[..., None])
    C_loc = jnp.einsum("bncs,bncsk,bncsv->bnckv", w_end, kc, vc)
    n_loc = jnp.einsum("bncs,bncsk->bnck", w_end, kc)

    def step(carry, inp):
        C, n, m = carry
        Cl, nl, ml, bl = inp
        m_new = jnp.maximum(bl + m, ml)
        a = jnp.exp(bl + m - m_new)
        c = jnp.exp(ml - m_new)
        C_new = a[..., None, None] * C + c[..., None, None] * Cl
        n_new = a[..., None] * n + c[..., None] * nl
        return (C_new, n_new, m_new), (C, n, m)

    init = (jnp.zeros((B, NH, D, D), f32), jnp.zeros((B, NH, D), f32), jnp.zeros((B, NH), f32))
    xs = (jnp.moveaxis(C_loc, 2, 0), jnp.moveaxis(n_loc, 2, 0),
          jnp.moveaxis(m_loc, 2, 0), jnp.moveaxis(b_last, 2, 0))
    _, (C_prev, n_prev, m_prev) = lax.scan(step, init, xs)
    C_prev = jnp.moveaxis(C_prev, 0, 2)
    n_prev = jnp.moveaxis(n_prev, 0, 2)
    m_prev = jnp.moveaxis(m_prev, 0, 2)

    log_inter = bcum + m_prev[..., None]
    m_j = jnp.maximum(log_inter, jnp.max(logD, axis=-1))
    w_inter = jnp.exp(log_inter - m_j)
    s = jnp.einsum("bncjd,bncsd->bncjs", qc, kc) * jnp.exp(logD - m_j[..., None])
    num = jnp.einsum("bncjs,bncsv->bncjv", s, vc) + \
        w_inter[..., None] * jnp.einsum("bncjk,bnckv->bncjv", qc, C_prev)
    den = jnp.sum(s, axis=-1) + w_inter * jnp.einsum("bncjk,bnck->bncj", qc, n_prev)
    h = num / jnp.maximum(jnp.abs(den), jnp.exp(-m_j))[..., None]
    h = jnp.transpose(h, (0, 2, 3, 1, 4)).reshape(B, S, NH, D)
    return h.astype(q.dtype)


def token_mixer(x, w_in, conv_w, conv_b, kv_norm_g, w_uk, w_uv, rel_bias, b_i, b_f,
                mh_norm_g, w_up_a, w_up_m, w_out):
    B, S, _ = x.shape
    h = x @ w_in
    (q_a, c_kv, q_idx, k_idx, w_idx, qk_m, v_m, i_m, f_m, o_m, g_a, g_m) = split_cols(h)
    c_kv = rms_norm(c_kv, kv_norm_g)
    o_a = dsa_attention(q_a.reshape(B, S, N_HEADS_A, HEAD_DIM_A), c_kv,
                        q_idx.reshape(B, S, N_HEADS_IDX, HEAD_DIM_IDX), k_idx, w_idx,
                        w_uk, w_uv, rel_bias).reshape(B, S, WIDTH_A)
    qk_m = jax.nn.silu(causal_conv(qk_m, conv_w, conv_b))
    q_m, k_m = jnp.split(qk_m, 2, axis=-1)
    hd = (B, S, N_HEADS_M, HEAD_DIM_M)
    h_m = mlstm(q_m.reshape(hd), k_m.reshape(hd), v_m.reshape(hd), i_m + b_i, f_m + b_f)
    h_m = jax.nn.sigmoid(o_m.reshape(hd)) * rms_norm(h_m, mh_norm_g)
    h_m = h_m.reshape(B, S, WIDTH_M)
    y = jax.nn.sigmoid(g_a) * (o_a @ w_up_a) + jax.nn.sigmoid(g_m) * (h_m @ w_up_m)
    return y @ w_out


def hier_moe(x, w_grp, b_grp, w_rt, b_rt, w_gate, w_up, w_down):
    B, S, D = x.shape
    t = x.reshape(-1, D)
    grp_prob = jax.nn.softmax((t @ w_grp + b_grp).astype(jnp.float32), axis=-1)
    g_w, g_idx = lax.top_k(grp_prob, 1)
    exp_logits = (t @ w_rt + b_rt).astype(jnp.float32).reshape(-1, N_GROUPS, EXPERTS_PER_GROUP)
    in_grp = jnp.take_along_axis(exp_logits, g_idx[:, :, None], axis=1)[:, 0]
    e_logit, e_idx = lax.top_k(in_grp, TOP_K_EXP)
    e_w = jax.nn.softmax(e_logit, axis=-1) * g_w
    glob = g_idx * EXPERTS_PER_GROUP + e_idx
    combine = jnp.sum(jax.nn.one_hot(glob, N_EXPERTS, dtype=jnp.float32) * e_w[..., None], axis=1)
    combine = combine.astype(t.dtype)
    out = jnp.zeros_like(t)
    for e in range(N_EXPERTS):
        hdn = jax.nn.silu(t @ w_gate[e]) * (t @ w_up[e])
        out = out + combine[:, e:e + 1] * (hdn @ w_down[e])
    return out.reshape(B, S, D)


def setup_inputs(seed: int = 0) -> dict:
    key = jax.random.key(seed)
    ks = jax.random.split(key, 26)
    n = lambda k, shape, s: jax.random.normal(k, shape, jnp.float32) * s
    L = DEPTH
    return {
        "x": n(ks[0], (BATCH, SEQ, D_MODEL), 1.0),
        "w_in": n(ks[1], (L, D_MODEL, D_IN), D_MODEL ** -0.5),
        "conv_w": n(ks[2], (L, CONV_WIDTH, 2 * WIDTH_M), CONV_WIDTH ** -0.5),
        "conv_b": n(ks[3], (L, 2 * WIDTH_M), 0.02),
        "kv_norm_g": 1.0 + n(ks[4], (L, KV_RANK), 0.02),
        "w_uk": n(ks[5], (L, KV_RANK, N_HEADS_A, HEAD_DIM_A), KV_RANK ** -0.5),
        "w_uv": n(ks[6], (L, KV_RANK, N_HEADS_A, HEAD_DIM_A), KV_RANK ** -0.5),
        "rel_bias": n(ks[7], (N_BUCKETS, N_HEADS_A), 0.5),
        "b_i": n(ks[8], (L, N_HEADS_M), 0.1),
        "b_f": jnp.linspace(3.0, 6.0, N_HEADS_M, dtype=jnp.float32)[None, :] + n(ks[9], (L, N_HEADS_M), 0.1),
        "mh_norm_g": 1.0 + n(ks[10], (L, N_HEADS_M, HEAD_DIM_M), 0.02),
        "w_up_a": n(ks[11], (L, WIDTH_A, D_MODEL), WIDTH_A ** -0.5),
        "w_up_m": n(ks[12], (L, WIDTH_M, D_MODEL), WIDTH_M ** -0.5),
        "w_out": n(ks[13], (L, D_MODEL, D_MODEL), BETA * D_MODEL ** -0.5),
        "ln1_g": 1.0 + n(ks[14], (L, D_MODEL), 0.02),
        "ln1_b": n(ks[15], (L, D_MODEL), 0.02),
        "w_grp": n(ks[16], (L, D_MODEL, N_GROUPS), D_MODEL ** -0.5),
        "b_grp": n(ks[17], (L, N_GROUPS), 0.01),
        "w_rt": n(ks[18], (L, D_MODEL, N_EXPERTS), D_MODEL ** -0.5),
        "b_rt": n(ks[19], (L, N_EXPERTS), 0.01),
        "w_gate": n(ks[20], (L, N_EXPERTS, D_MODEL, D_EXPERT), D_MODEL ** -0.5),
        "w_up": n(ks[21], (L, N_EXPERTS, D_MODEL, D_EXPERT), D_MODEL ** -0.5),
        "w_down": n(ks[22], (L, N_EXPERTS, D_EXPERT, D_MODEL), BETA * D_EXPERT ** -0.5),
        "ln2_g": 1.0 + n(ks[23], (L, D_MODEL), 0.02),
        "ln2_b": n(ks[24], (L, D_MODEL), 0.02),
    }


def reference(x, w_in, conv_w, conv_b, kv_norm_g, w_uk, w_uv, rel_bias, b_i, b_f,
              mh_norm_g, w_up_a, w_up_m, w_out, ln1_g, ln1_b, w_grp, b_grp, w_rt, b_rt,
              w_gate, w_up, w_down, ln2_g, ln2_b):
    for l in range(DEPTH):
        mix = token_mixer(x, w_in[l], conv_w[l], conv_b[l], kv_norm_g[l], w_uk[l], w_uv[l],
                          rel_bias, b_i[l], b_f[l], mh_norm_g[l], w_up_a[l], w_up_m[l], w_out[l])
        x = layer_norm(ALPHA * x + mix, ln1_g[l], ln1_b[l])
        ffn = hier_moe(x, w_grp[l], b_grp[l], w_rt[l], b_rt[l], w_gate[l], w_up[l], w_down[l])
        x = layer_norm(ALPHA * x + ffn, ln2_g[l], ln2_b[l])
    return x
```

```python
import math
import os
from contextlib import ExitStack

import numpy as np
import concourse.bass as bass
import concourse.mybir as mybir
from concourse.bass_utils import run_bass_kernel_spmd

F32 = mybir.dt.float32
BF16 = mybir.dt.bfloat16
AF = mybir.ActivationFunctionType
ALU = mybir.AluOpType
AX = mybir.AxisListType

NCORES = 8
_DBG = {}
T = 4096
S = 2048
D = 1024
D_IN = 5456
ALPHA = 2.0 ** 0.25
LN_EPS = 1e-5
NEG = -1.0e30
NBIS = 20
SCW = 0.0
C_QA, C_CKV, C_QI, C_KI, C_WI, C_QKM, C_VM, C_IM, C_FM, C_OM, C_GA, C_GM = (
    0, 512, 768, 1280, 1344, 1352, 2376, 2888, 2892, 2896, 3408, 4432)


class Buf:
    __slots__ = ("t", "w", "r")

    def __init__(self, t):
        self.t = t
        self.w = {}
        self.r = {}


class TK:
    def __init__(self, nc, es):
        self.nc = nc
        self.E = {"pe": nc.tensor, "act": nc.scalar, "dve": nc.vector, "pool": nc.gpsimd, "sp": nc.sync}
        self.sem = {}
        self.cnt = {}
        for e in ("pe", "act", "dve", "pool"):
            self.sem[e] = es.enter_context(nc.semaphore("s_" + e))
            self.cnt[e] = 0
        self.dq = {"sp": [], "pool": []}
        self.dqi = {"sp": 0, "pool": 0}
        for q, n in (("sp", 8), ("pool", 6)):
            for i in range(n):
                nm = f"d_{q}{i}"
                self.sem[nm] = es.enter_context(nc.semaphore(nm))
                self.cnt[nm] = 0
                self.dq[q].append(nm)
        self.seen = {e: {} for e in self.E}

    def wait(self, e, deps):
        for s, v in deps.items():
            if e == "pe" and s == "pe":
                continue
            if self.seen[e].get(s, 0) >= v:
                continue
            self.E[e].wait_ge(self.sem[s], v)
            self.seen[e][s] = v

    @staticmethod
    def _merge(d, src):
        for s, v in src.items():
            if d.get(s, 0) < v:
                d[s] = v

    def _deps(self, reads, writes):
        deps = {}
        for b in reads:
            self._merge(deps, b.w)
        for b in writes:
            self._merge(deps, b.w)
            self._merge(deps, b.r)
        return deps

    def _post(self, s, v, reads, writes):
        for b in reads:
            b.r[s] = v
        for b in writes:
            b.w = {s: v}
            b.r = {}

    def op(self, e, method, *args, reads=(), writes=(), **kw):
        self.wait(e, self._deps(reads, writes))
        ins = getattr(self.E[e], method)(*args, **kw)
        self.cnt[e] += 1
        ins.then_inc(self.sem[e], 1)
        self._post(e, self.cnt[e], reads, writes)

    def dma(self, q, out, in_, reads=(), writes=()):
        nm = self.dq[q][self.dqi[q] % len(self.dq[q])]
        self.dqi[q] += 1
        deps = self._deps(reads, writes)
        if self.cnt[nm] > 0:
            deps[nm] = max(deps.get(nm, 0), self.cnt[nm])
        self.wait(q, deps)
        self.E[q].dma_start(out=out, in_=in_).then_inc(self.sem[nm], 16)
        self.cnt[nm] += 16
        self._post(nm, self.cnt[nm], reads, writes)

    def barrier(self):
        allv = {s: v for s, v in self.cnt.items() if v > 0}
        for e in self.E:
            self.wait(e, dict(allv))

    def finish(self):
        allv = {s: v for s, v in self.cnt.items() if v > 0 and s.startswith("d_")}
        self.wait("sp", allv)


def build_nc(stop=99, debug=False, ntile=16, sub=99, ngroups=8, nchunks=16):
    nc = bass.Bass("TRN2", target_bir_lowering=False)

    def din(name, shape, dt=F32):
        return nc.dram_tensor(name, shape, dt, kind="ExternalInput").ap()

    def dscr(name, shape, dt):
        return nc.dram_tensor(name, shape, dt, kind="ExternalOutput" if debug else "Internal").ap()

    x = din("x", [T, D])
    w_in = din("w_in", [D, D_IN])
    conv_w = din("conv_w", [128, 8, 4])
    conv_b = din("conv_b", [128, 8, 1])
    kvg = din("kvg", [128, 2, 1])
    w_uk = din("w_uk", [256, 512])
    w_uv = din("w_uv", [256, 512])
    relb = din("relb", [32, 8])
    relb31 = din("relb31", [32, 8])
    oh = din("oh", [32, 2, 128, 128])
    bif = din("bif", [128, 8])
    mhg = din("mhg", [128, 512])
    w_up_a = din("w_up_a", [512, D])
    w_up_m = din("w_up_m", [512, D])
    w_out = din("w_out", [D, D])
    ln1g = din("ln1g", [128, D])
    ln1b = din("ln1b", [128, D])
    ln2g = din("ln2g", [128, D])
    ln2b = din("ln2b", [128, D])
    w_r = din("w_r", [D, 20])
    b_r = din("b_r", [128, 20])
    w_gate = din("w_gate", [16, D, 512])
    w_up = din("w_up", [16, D, 512])
    w_down = din("w_down", [16, 512, D])
    c_ident = din("c_ident", [128, 128])
    c_tri = din("c_tri", [128, 128])
    c_trineg = din("c_trineg", [128, 128])
    c_ones = din("c_ones", [128, 128])
    c_ck = din("c_ck", [128, NBIS + 1])
    out = nc.dram_tensor("out", [T, D], F32, kind="ExternalOutput").ap()

    QA = dscr("QA", [8, 64, T], BF16)
    QI = dscr("QI", [8, 64, T], BF16)
    KI = dscr("KI", [64, T], BF16)
    KT = dscr("KT", [8, 64, T], BF16)
    QKM = dscr("QKM", [8, 128, T], BF16)
    GT = dscr("GT", [16, 128, T], BF16)
    VA = dscr("VA", [T, 520], BF16)
    VM = dscr("VM", [T, 516], BF16)
    OM = dscr("OM", [T, 512], BF16)
    SM = dscr("SM", [T, 32], F32)
    HM = dscr("HM", [T, 512], BF16)
    X1 = dscr("X1", [T, D], F32)
    X1T = dscr("X1T", [8, 128, T], BF16)
    CMB = dscr("CMB", [T, 16], F32)

    with ExitStack() as es:
        tk = TK(nc, es)

        uid = [0]

        def mk(stack):
            def sb(name, shape, dt):
                uid[0] += 1
                return Buf(stack.enter_context(nc.sbuf_tensor(f"{name}_{uid[0]}", shape, dt)))

            def ps(name, shape, dt):
                uid[0] += 1
                return Buf(stack.enter_context(nc.psum_tensor(f"{name}_{uid[0]}", shape, dt)))
            return sb, ps

        gsb, _ = mk(es)
        identf = gsb("identf", [128, 128], F32)
        identb = gsb("identb", [128, 128], BF16)
        trif = gsb("trif", [128, 128], F32)
        trib = gsb("trib", [128, 128], BF16)
        trineg = gsb("trineg", [128, 128], F32)
        onesf = gsb("onesf", [128, 128], F32)
        onesb = gsb("onesb", [128, 128], BF16)
        epsc = gsb("epsc", [128, 1], F32)
        erel = gsb("erel", [128, 2, 8, 128], BF16)
        tk.dma("sp", identf.t[:], c_ident, writes=[identf])
        tk.dma("pool", identb.t[:], c_ident, writes=[identb])
        tk.dma("sp", trif.t[:], c_tri, writes=[trif])
        tk.dma("pool", trib.t[:], c_tri, writes=[trib])
        tk.dma("sp", trineg.t[:], c_trineg, writes=[trineg])
        tk.dma("sp", onesf.t[:], c_ones, writes=[onesf])
        tk.dma("pool", onesb.t[:], c_ones, writes=[onesb])
        tk.op("dve", "memset", epsc.t[:], LN_EPS, writes=[epsc])
        ckc = gsb("ckc", [128, NBIS + 1], F32)
        tk.dma("sp", ckc.t[:], c_ck, writes=[ckc])

        with ExitStack() as s0:
            sb, ps = mk(s0)
            rb = sb("rb", [32, 8], F32)
            rb31 = sb("rb31", [32, 8], F32)
            rba = sb("rba", [32, 8], F32)
            tk.dma("sp", rb.t[:], relb, writes=[rb])
            tk.dma("sp", rb31.t[:], relb31, writes=[rb31])
            tk.op("dve", "tensor_tensor", rba.t[:], rb.t[:], rb31.t[:], ALU.subtract, reads=[rb, rb31], writes=[rba])
            ohc = [sb(f"ohc{i}", [32, 32, 128], F32) for i in range(2)]
            pe_ = [ps(f"pe{i}", [128, 256], F32) for i in range(2)]
            n = 0
            for blk in range(2):
                for tc_ in range(4):
                    o = ohc[n % 2]
                    p = pe_[n % 2]
                    n += 1
                    tk.dma("sp", o.t[:], oh[:, blk, tc_ * 32:(tc_ + 1) * 32, :], writes=[o])
                    for tl in range(32):
                        tk.op("pe", "matmul", p.t[:, tl * 8:(tl + 1) * 8], o.t[:, tl, :], rba.t[:, :],
                              start=True, stop=True, reads=[o, rba], writes=[p])
                    tk.op("act", "activation", out=erel.t[:, blk, :, tc_ * 32:(tc_ + 1) * 32],
                          in_=p.t[:, :].rearrange("p (t h) -> p h t", h=8), func=AF.Exp,
                          reads=[p], writes=[erel])
        tk.barrier()

        with ExitStack() as s1:
            if stop < 1:
                tk.finish()
                return nc
            sb, ps = mk(s1)
            win = [sb(f"win{k}", [128, D_IN], BF16) for k in range(8)]
            xb = [sb(f"xb{i}", [128, 4, D], BF16) for i in range(2)]

            def load_x(g):
                tk.dma("pool", xb[g % 2].t[:], x[g * 512:(g + 1) * 512, :].rearrange("(j p) c -> p j c", p=128), writes=[xb[g % 2]])

            load_x(0)
            SEGS = [C_QA, C_CKV, C_QI, C_KI, C_WI, C_QKM, C_VM, C_IM, C_OM, C_GA, C_GM, D_IN]
            wseg = [[Buf(win[k].t[:, SEGS[si]:SEGS[si + 1]]) for si in range(len(SEGS) - 1)] for k in range(8)]
            for si in (0, 2, 3, 1, 5, 9, 10, 4, 7, 6, 8):
                for k in range(8):
                    tk.dma("pool", wseg[k][si].t, w_in[k * 128:(k + 1) * 128, SEGS[si]:SEGS[si + 1]], writes=[wseg[k][si]])

            def wsl(k, c0, n):
                for si in range(len(SEGS) - 1):
                    if SEGS[si] <= c0 and c0 + n <= SEGS[si + 1]:
                        return win[k].t[:, c0:c0 + n], wseg[k][si]
                raise AssertionError((c0, n))
            cw = sb("cw", [128, 8, 4], F32)
            cb = sb("cb", [128, 8, 1], F32)
            tk.dma("sp", cw.t[:], conv_w, writes=[cw])
            tk.dma("sp", cb.t[:], conv_b, writes=[cb])
            kg = sb("kg", [128, 2, 1], F32)
            tk.dma("sp", kg.t[:], kvg, writes=[kg])
            wkf = sb("wkf", [128, 2, 512], F32)
            wvf = sb("wvf", [128, 2, 512], F32)
            tk.dma("sp", wkf.t[:], w_uk.rearrange("(m p) c -> p m c", p=128), writes=[wkf])
            tk.dma("sp", wvf.t[:], w_uv.rearrange("(m p) c -> p m c", p=128), writes=[wvf])
            wukg = sb("wukg", [128, 2, 512], BF16)
            wuvg = sb("wuvg", [128, 2, 512], BF16)
            for rc in range(2):
                tk.op("dve", "tensor_scalar", wukg.t[:, rc, :], wkf.t[:, rc, :], kg.t[:, rc, 0:1], None, ALU.mult,
                      reads=[wkf, kg], writes=[wukg])
                tk.op("dve", "tensor_scalar", wuvg.t[:, rc, :], wvf.t[:, rc, :], kg.t[:, rc, 0:1], None, ALU.mult,
                      reads=[wvf, kg], writes=[wuvg])
            bifs = sb("bifs", [128, 8], F32)
            tk.dma("sp", bifs.t[:], bif, writes=[bifs])

            xT = [sb(f"xT{i}", [128, 8, 512], BF16) for i in range(2)]
            crT = [sb(f"crT{i}", [128, 2, 512], BF16) for i in range(2)]
            U = [sb(f"U{m}", [128, 515], F32) for m in range(8)]
            cacc = [sb(f"cacc{i}", [128, 512], F32) for i in range(2)]
            stg = [sb(f"stg{i}", [128, 512], BF16) for i in range(10)]
            vms = [sb(f"vms{i}", [128, 4, 129], BF16) for i in range(2)]
            vas = [sb(f"vas{i}", [128, 8, 65], BF16) for i in range(2)]
            sms = [sb(f"sms{i}", [128, 32], F32) for i in range(3)]
            junk = sb("junk", [128, 256], F32)
            ssq = sb("ssq", [128, 1], F32)
            ftmp = sb("ftmp", [128, 4], F32)
            pT = [ps(f"pT{i}", [128, 512], BF16) for i in range(2)]
            pp = [ps(f"pp{i}", [128, 512], F32) for i in range(6)]
            for v in vms:
                tk.op("pool", "memset", v.t[:], 1.0, writes=[v])
            for v in vas:
                tk.op("pool", "memset", v.t[:], 1.0, writes=[v])
            for s_ in sms:
                tk.op("pool", "memset", s_.t[:], 0.0, writes=[s_])
            cnt = {"pp": 0, "stg": 0, "ev": 0}

            def nextpp():
                cnt["pp"] += 1
                return pp[cnt["pp"] % 6]

            def nextstg():
                cnt["stg"] += 1
                return stg[cnt["stg"] % 10]

            def evac_copy(dst_ap, src_ap, reads, writes):
                cnt["ev"] += 1
                if cnt["ev"] % 2:
                    tk.op("act", "copy", dst_ap, src_ap, reads=reads, writes=writes)
                else:
                    tk.op("dve", "tensor_copy", dst_ap, src_ap, reads=reads, writes=writes)

            for g in range(ngroups):
                t0 = g * 512
                xbuf = xb[g % 2]
                xt_ = xT[g % 2]
                cr = crT[g % 2]
                for k in range(8):
                    pt = pT[k % 2]
                    for j in range(4):
                        tk.op("pe", "transpose", pt.t[:, j * 128:(j + 1) * 128], xbuf.t[:, j, k * 128:(k + 1) * 128],
                              identb.t[:], reads=[xbuf, identb], writes=[pt])
                    evac_copy(xt_.t[:, k, :], pt.t[:, :], [pt], [xt_])
                if g + 1 < ngroups:
                    load_x(g + 1)

                def fm(c0, M):
                    p = nextpp()
                    for k in range(8):
                        wap, wbuf = wsl(k, c0, M)
                        tk.op("pe", "matmul", p.t[:M, :], wap, xt_.t[:, k, :],
                              start=(k == 0), stop=(k == 7), reads=[wbuf, xt_], writes=[p])
                    return p

                for h in range(8):
                    p = fm(C_QA + h * 64, 64)
                    st = nextstg()
                    evac_copy(st.t[:64, :], p.t[:64, :], [p], [st])
                    tk.dma("sp", QA[h, :, t0:t0 + 512], st.t[:64, :], reads=[st])
                for h in range(8):
                    p = fm(C_QI + h * 64, 64)
                    st = nextstg()
                    evac_copy(st.t[:64, :], p.t[:64, :], [p], [st])
                    tk.dma("sp", QI[h, :, t0:t0 + 512], st.t[:64, :], reads=[st])
                p = fm(C_KI, 64)
                st = nextstg()
                evac_copy(st.t[:64, :], p.t[:64, :], [p], [st])
                tk.dma("sp", KI[:, t0:t0 + 512], st.t[:64, :], reads=[st])
                for rc in range(2):
                    p = fm(C_CKV + rc * 128, 128)
                    evac_copy(cr.t[:, rc, :], p.t[:, :], [p], [cr])
                for h in range(8):
                    p = nextpp()
                    for rc in range(2):
                        tk.op("pe", "matmul", p.t[:64, :], wukg.t[:, rc, h * 64:(h + 1) * 64], cr.t[:, rc, :],
                              start=(rc == 0), stop=(rc == 1), reads=[wukg, cr], writes=[p])
                    st = nextstg()
                    evac_copy(st.t[:64, :], p.t[:64, :], [p], [st])
                    tk.dma("sp", KT[h, :, t0:t0 + 512], st.t[:64, :], reads=[st])
                for m in range(8):
                    p = fm(C_QKM + m * 128, 128)
                    u = U[m]
                    if g % 4 == 0:
                        tk.op("dve", "memset", u.t[:, 0:3], 0.0, writes=[u])
                    tk.op("act", "copy", u.t[:, 3:515], p.t[:, :], reads=[p], writes=[u])
                    ca = cacc[m % 2]
                    tk.op("dve", "tensor_scalar", ca.t[:, :], u.t[:, 0:512], cw.t[:, m, 0:1], None, ALU.mult,
                          reads=[u, cw], writes=[ca])
                    for j in range(1, 4):
                        tk.op("dve", "scalar_tensor_tensor", ca.t[:, :], u.t[:, j:j + 512], cw.t[:, m, j:j + 1], ca.t[:, :],
                              ALU.mult, ALU.add, reads=[u, cw], writes=[ca])
                    st = nextstg()
                    tk.op("act", "activation", out=st.t[:, :], in_=ca.t[:, :], func=AF.Silu, bias=cb.t[:, m, 0:1],
                          reads=[ca, cb], writes=[st])
                    tk.dma("sp", QKM[m, :, t0:t0 + 512], st.t[:, :], reads=[st])
                    tk.op("dve", "tensor_copy", u.t[:, 0:3], u.t[:, 512:515], writes=[u])
                for m in range(16):
                    p = fm(C_GA + m * 128, 128)
                    st = nextstg()
                    tk.op("act", "activation", out=st.t[:, :], in_=p.t[:, :], func=AF.Sigmoid, reads=[p], writes=[st])
                    tk.dma("sp", GT[m, :, t0:t0 + 512], st.t[:, :], reads=[st])
                for j in range(4):
                    r0 = t0 + j * 128
                    xs = lambda k: xt_.t[:, k, j * 128:(j + 1) * 128]
                    pa = nextpp()
                    for (o0, c0, nw) in ((0, C_CKV, 256), (256, C_WI, 8), (264, C_IM, 8)):
                        for k in range(8):
                            wap, wbuf = wsl(k, c0, nw)
                            tk.op("pe", "matmul", pa.t[:, o0:o0 + nw], xs(k), wap,
                                  start=(k == 0), stop=(k == 7), reads=[wbuf, xt_], writes=[pa])
                    sm = sms[(g * 4 + j) % 3]
                    tk.op("act", "activation", out=junk.t[:, :], in_=pa.t[:, 0:256], func=AF.Square, accum_out=ssq.t[:, 0:1],
                          reads=[pa], writes=[junk, ssq])
                    tk.op("act", "activation", out=sm.t[:, 16:17], in_=ssq.t[:, :], func=AF.Sqrt, scale=1.0 / 256.0, bias=epsc.t[:, 0:1],
                          reads=[ssq, epsc], writes=[sm])
                    tk.op("dve", "reciprocal", sm.t[:, 16:17], sm.t[:, 16:17], writes=[sm])
                    tk.op("dve", "tensor_copy", sm.t[:, 0:8], pa.t[:, 256:264], reads=[pa], writes=[sm])
                    tk.op("dve", "tensor_tensor", sm.t[:, 8:12], pa.t[:, 264:268], bifs.t[:, 0:4], ALU.add,
                          reads=[pa, bifs], writes=[sm])
                    tk.op("dve", "tensor_tensor", ftmp.t[:, :], pa.t[:, 268:272], bifs.t[:, 4:8], ALU.add,
                          reads=[pa, bifs], writes=[ftmp])
                    tk.op("act", "activation", out=ftmp.t[:, :], in_=ftmp.t[:, :], func=AF.Exp, scale=-1.0, writes=[ftmp])
                    tk.op("dve", "tensor_scalar", ftmp.t[:, :], ftmp.t[:, :], 1.0, None, ALU.add, writes=[ftmp])
                    tk.op("act", "activation", out=ftmp.t[:, :], in_=ftmp.t[:, :], func=AF.Ln, writes=[ftmp])
                    tk.op("dve", "tensor_scalar", sm.t[:, 12:16], ftmp.t[:, :], -1.0, None, ALU.mult, reads=[ftmp], writes=[sm])
                    tk.dma("sp", SM[r0:r0 + 128, :], sm.t[:, :], reads=[sm])
                    pv = nextpp()
                    for k in range(8):
                        wap, wbuf = wsl(k, C_VM, 512)
                        tk.op("pe", "matmul", pv.t[:, :], xs(k), wap,
                              start=(k == 0), stop=(k == 7), reads=[wbuf, xt_], writes=[pv])
                    vm_ = vms[j % 2]
                    tk.op("act", "copy", vm_.t[:, :, 0:128], pv.t[:, :].rearrange("p (h d) -> p h d", h=4),
                          reads=[pv], writes=[vm_])
                    tk.dma("sp", VM[r0:r0 + 128, :], vm_.t[:, :, :].rearrange("p h d -> p (h d)"), reads=[vm_])
                    po = nextpp()
                    for k in range(8):
                        wap, wbuf = wsl(k, C_OM, 512)
                        tk.op("pe", "matmul", po.t[:, :], xs(k), wap,
                              start=(k == 0), stop=(k == 7), reads=[wbuf, xt_], writes=[po])
                    st = nextstg()
                    tk.op("act", "activation", out=st.t[:, :], in_=po.t[:, :], func=AF.Sigmoid, reads=[po], writes=[st])
                    tk.dma("sp", OM[r0:r0 + 128, :], st.t[:, :], reads=[st])
                    pv2 = nextpp()
                    for rc in range(2):
                        tk.op("pe", "matmul", pv2.t[:, :], cr.t[:, rc, j * 128:(j + 1) * 128], wuvg.t[:, rc, :],
                              start=(rc == 0), stop=(rc == 1), reads=[cr, wuvg], writes=[pv2])
                    va_ = vas[j % 2]
                    tk.op("dve", "tensor_scalar", va_.t[:, :, 0:64], pv2.t[:, :].rearrange("p (h d) -> p h d", h=8),
                          sm.t[:, 16:17], None, ALU.mult, reads=[pv2, sm], writes=[va_])
                    tk.dma("sp", VA[r0:r0 + 128, :], va_.t[:, :, :].rearrange("p h d -> p (h d)"), reads=[va_])
        tk.barrier()
        if stop < 3:
            tk.finish()
            return nc

        for b in range(2):
            b0 = b * S
            s34 = ExitStack()
            sbo, _ = mk(s34)
            KTs = [sbo(f"KTs{h}", [64, S], BF16) for h in range(8)]
            VAs = sbo("VAs", [128, 16, 520], BF16)
            KIs = sbo("KIs", [64, S], BF16)
            smb = sbo("smb4", [128, 16, 32], F32)
            rs = sbo("rs", [128, 16], F32)
            wua = sbo("wua", [128, 4, D], BF16)
            wum = sbo("wum", [128, 4, D], BF16)
            wo = sbo("wo", [128, 8, D], BF16)
            wrs = sbo("wrs", [128, 8, 20], F32)
            brs = sbo("brs", [128, 20], F32)
            g1 = sbo("g1", [128, D], F32)
            b1 = sbo("b1", [128, D], F32)

            def issue_s4_loads():
                for h in range(8):
                    tk.dma("sp", KTs[h].t[:], KT[h, :, b0:b0 + S], writes=[KTs[h]])
                tk.dma("sp", VAs.t[:], VA[b0:b0 + S, :].rearrange("(c p) f -> p c f", p=128), writes=[VAs])
                tk.dma("sp", KIs.t[:], KI[:, b0:b0 + S], writes=[KIs])
                tk.dma("sp", smb.t[:], SM[b0:b0 + S, :].rearrange("(c p) f -> p c f", p=128), writes=[smb])
                tk.op("dve", "tensor_scalar", rs.t[:, :], smb.t[:, :, 16], 0.125, None, ALU.mult, reads=[smb], writes=[rs])
                tk.dma("pool", wua.t[:], w_up_a.rearrange("(k p) c -> p k c", p=128), writes=[wua])
                tk.dma("pool", wum.t[:], w_up_m.rearrange("(k p) c -> p k c", p=128), writes=[wum])
                tk.dma("pool", wo.t[:], w_out.rearrange("(k p) c -> p k c", p=128), writes=[wo])
                tk.dma("sp", wrs.t[:], w_r.rearrange("(k p) c -> p k c", p=128), writes=[wrs])
                tk.dma("sp", brs.t[:], b_r, writes=[brs])
                tk.dma("sp", g1.t[:], ln1g, writes=[g1])
                tk.dma("sp", b1.t[:], ln1b, writes=[b1])

            with ExitStack() as s3:
                sb, ps = mk(s3)
                qkT = [sb(f"qkT{m}", [128, S], BF16) for m in range(8)]
                for m in range(8):
                    tk.dma("sp", qkT[m].t[:], QKM[m, :, b0:b0 + S], writes=[qkT[m]])
                vmb = sb("vmb", [128, 16, 516], BF16)
                omb = sb("omb", [128, 16, 512], BF16)
                smb3 = sb("smb3", [128, 16, 32], F32)
                tk.dma("sp", vmb.t[:], VM[b0:b0 + S, :].rearrange("(c p) f -> p c f", p=128), writes=[vmb])
                tk.dma("sp", omb.t[:], OM[b0:b0 + S, :].rearrange("(c p) f -> p c f", p=128), writes=[omb])
                tk.dma("sp", smb3.t[:], SM[b0:b0 + S, :].rearrange("(c p) f -> p c f", p=128), writes=[smb3])
                mhgs = sb("mhgs", [128, 512], F32)
                tk.dma("sp", mhgs.t[:], mhg, writes=[mhgs])
                issue_s4_loads()
                hmo = sb("hmo", [128, 16, 512], BF16)
                lf = sb("lf", [128, 64], F32)
                ii = sb("ii", [128, 64], F32)
                bc = sb("bc", [128, 64], F32)
                bt = sb("bt", [128, 64], F32)
                d1 = sb("d1", [128, 64], F32)
                d2 = sb("d2", [128, 64], F32)
                a1s = sb("a1s", [128, 64], F32)
                a2s = sb("a2s", [128, 64], F32)
                et = sb("et", [128, 64], F32)
                eb = sb("eb", [128, 64], F32)
                hbk = [ps(f"hbk{h}", [128, 512], F32) for h in range(4)]
                class _V:
                    def __init__(self, ap):
                        self.t = ap
                pST = [_V(hbk[h].t[:, 0:128]) for h in range(4)]
                pN = [_V(hbk[h].t[:, 128:257]) for h in range(4)]
                pCU = [_V(hbk[h].t[:, 257:386]) for h in range(4)]
                pKTb = ps("pKTb", [128, 512], BF16)
                pKT = [_V(pKTb.t[:, h * 128:(h + 1) * 128]) for h in range(4)]
                pg, pg2 = pST[0], pST[1]
                tk.op("dve", "tensor_copy", lf.t[:, :].rearrange("p (c h) -> p c h", h=4), smb3.t[:, :, 12:16], reads=[smb3], writes=[lf])
                tk.op("dve", "tensor_copy", ii.t[:, :].rearrange("p (c h) -> p c h", h=4), smb3.t[:, :, 8:12], reads=[smb3], writes=[ii])
                tk.op("pe", "matmul", pg.t[:, 0:64], trif.t[:, :], lf.t[:, :], start=True, stop=True, reads=[trif, lf], writes=[hbk[0]])
                tk.op("pe", "matmul", pg2.t[:, 0:64], onesf.t[:, :], lf.t[:, :], start=True, stop=True, reads=[onesf, lf], writes=[hbk[1]])
                tk.op("dve", "tensor_copy", bc.t[:, :], pg.t[:, 0:64], reads=[hbk[0]], writes=[bc])
                tk.op("dve", "tensor_copy", bt.t[:, :], pg2.t[:, 0:64], reads=[hbk[1]], writes=[bt])
                tk.op("dve", "tensor_tensor", d1.t[:, :], ii.t[:, :], bc.t[:, :], ALU.subtract, reads=[ii, bc], writes=[d1])
                tk.op("dve", "tensor_tensor", d2.t[:, :], d1.t[:, :], bt.t[:, :], ALU.add, reads=[d1, bt], writes=[d2])
                tk.op("act", "activation", out=a1s.t[:, :], in_=d1.t[:, :], func=AF.Exp, reads=[d1], writes=[a1s])
                tk.op("act", "activation", out=a2s.t[:, :], in_=d2.t[:, :], func=AF.Exp, reads=[d2], writes=[a2s])
                tk.op("act", "activation", out=et.t[:, :], in_=bc.t[:, :], func=AF.Exp, reads=[bc], writes=[et])
                tk.op("act", "activation", out=eb.t[:, :], in_=bt.t[:, :], func=AF.Exp, reads=[bt], writes=[eb])
                isd = 128.0 ** -0.5
                tk.op("dve", "tensor_scalar", a1s.t[:, :], a1s.t[:, :], isd, None, ALU.mult, writes=[a1s])
                tk.op("dve", "tensor_scalar", a2s.t[:, :], a2s.t[:, :], isd, None, ALU.mult, writes=[a2s])
                Cf = [sb(f"Cf{h}", [128, 129], F32) for h in range(4)]
                Cb = [sb(f"Cb{h}", [128, 129], BF16) for h in range(4)]
                for h in range(4):
                    tk.op("pool", "memset", Cf[h].t[:], 0.0, writes=[Cf[h]])
                STs = [sb(f"STs{i}", [128, 128], BF16) for i in range(4)]
                kss = [sb(f"kss{i}", [128, 128], BF16) for i in range(4)]
                hf = [sb(f"hf{i}", [128, 4, 128], F32) for i in range(2)]
                hj = [sb(f"hj{i}", [128, 4, 128], F32) for i in range(2)]
                hn = [sb(f"hn{i}", [128, 4, 128], F32) for i in range(2)]
                dn = [sb(f"dn{i}", [128, 4, 4], F32) for i in range(2)]
                for c in range(nchunks):
                    cs = slice(c * 128, (c + 1) * 128)
                    c4 = slice(c * 4, (c + 1) * 4)
                    hfc, hjc, hnc, dd = hf[c % 2], hj[c % 2], hn[c % 2], dn[c % 2]
                    for h in range(4):
                        tk.op("pe", "matmul", pST[h].t[:, :], qkT[4 + h].t[:, cs], qkT[h].t[:, cs], start=True, stop=True,
                              reads=[qkT[4 + h], qkT[h]], writes=[hbk[h]])
                    for h in range(4):
                        ch = c * 4 + h
                        tk.op("dve", "scalar_tensor_tensor", STs[h].t[:, :], pST[h].t[:, :], a1s.t[:, ch:ch + 1], trib.t[:, :],
                              ALU.mult, ALU.mult, reads=[hbk[h], a1s, trib], writes=[STs[h]])
                    for h in range(4):
                        vch = vmb.t[:, c, h * 129:(h + 1) * 129]
                        tk.op("pe", "matmul", pN[h].t[:, :], STs[h].t[:, :], vch, start=True, stop=(c == 0),
                              reads=[STs[h], vmb], writes=[hbk[h]])
                        if c > 0:
                            tk.op("pe", "matmul", pN[h].t[:, :], qkT[h].t[:, cs], Cb[h].t[:, :], start=False, stop=True,
                                  reads=[qkT[h], Cb[h]], writes=[hbk[h]])
                    if c < 15:
                        for h in range(4):
                            tk.op("pe", "transpose", pKT[h].t[:, :], qkT[4 + h].t[:, cs], identb.t[:, :], reads=[qkT[4 + h], identb], writes=[pKTb])
                        for h in range(4):
                            ch = c * 4 + h
                            tk.op("act", "activation", out=kss[h].t[:, :], in_=pKT[h].t[:, :], func=AF.Copy, scale=a2s.t[:, ch:ch + 1],
                                  reads=[pKTb, a2s], writes=[kss[h]])
                        for h in range(4):
                            vch = vmb.t[:, c, h * 129:(h + 1) * 129]
                            tk.op("pe", "matmul", pCU[h].t[:, :], kss[h].t[:, :], vch, start=True, stop=True,
                                  reads=[kss[h], vmb], writes=[hbk[h]])
                        for h in range(4):
                            ch = c * 4 + h
                            tk.op("dve", "scalar_tensor_tensor", Cf[h].t[:, :], Cf[h].t[:, :], eb.t[:, ch:ch + 1], pCU[h].t[:, :],
                                  ALU.mult, ALU.add, reads=[eb, hbk[h]], writes=[Cf[h]])
                        for h in range(4):
                            tk.op("act", "copy", Cb[h].t[:, :], Cf[h].t[:, :], reads=[Cf[h]], writes=[Cb[h]])
                    for h in range(4):
                        ch = c * 4 + h
                        tk.op("act", "activation", out=dd.t[:, 0, h:h + 1], in_=pN[h].t[:, 128:129], func=AF.Abs, scale=et.t[:, ch:ch + 1],
                              reads=[hbk[h], et], writes=[dd])
                    tk.op("dve", "tensor_scalar", dd.t[:, 0, :], dd.t[:, 0, :], 1.0, None, ALU.max, writes=[dd])
                    tk.op("dve", "reciprocal", dd.t[:, 1, :], dd.t[:, 0, :], writes=[dd])
                    tk.op("dve", "tensor_tensor", dd.t[:, 2, :], dd.t[:, 1, :], et.t[:, c4], ALU.mult, reads=[et], writes=[dd])
                    for h in range(4):
                        tk.op("act", "activation", out=hfc.t[:, h, :], in_=pN[h].t[:, 0:128], func=AF.Copy, scale=dd.t[:, 2, h:h + 1],
                              reads=[hbk[h], dd], writes=[hfc])
                    tk.op("act", "activation", out=hjc.t[:, :, :], in_=hfc.t[:, :, :], func=AF.Square, reads=[hfc], writes=[hjc])
                    tk.op("dve", "tensor_reduce", dd.t[:, 3, :], hjc.t[:, :, :], AX.X, ALU.add, reads=[hjc], writes=[dd])
                    tk.op("act", "activation", out=dd.t[:, 3, :], in_=dd.t[:, 3, :], func=AF.Sqrt, scale=1.0 / 128.0, bias=epsc.t[:, 0:1],
                          reads=[epsc], writes=[dd])
                    tk.op("dve", "reciprocal", dd.t[:, 3, :], dd.t[:, 3, :], writes=[dd])
                    tk.op("dve", "tensor_tensor", hnc.t[:, :, :], hfc.t[:, :, :], mhgs.t[:, :].rearrange("p (h d) -> p h d", h=4), ALU.mult,
                          reads=[hfc, mhgs], writes=[hnc])
                    tk.op("dve", "tensor_tensor", hnc.t[:, :, :], hnc.t[:, :, :], dd.t[:, 3, :].unsqueeze(2).broadcast_to([128, 4, 128]), ALU.mult,
                          reads=[dd], writes=[hnc])
                    tk.op("pool", "tensor_tensor", hmo.t[:, c, :], hnc.t[:, :, :].rearrange("p h d -> p (h d)"), omb.t[:, c, :],
                          ALU.mult, reads=[hnc, omb], writes=[hmo])
                tk.dma("sp", HM[b0:b0 + S, :].rearrange("(c p) f -> p c f", p=128), hmo.t[:], reads=[hmo])
            tk.barrier()
            if stop < 4:
                tk.finish()
                return nc

            with ExitStack() as s4:
                sb, ps = mk(s4)
                QIt = [sb(f"QIt{i}", [64, 8, 128], BF16) for i in range(2)]
                QAt = [sb(f"QAt{i}", [64, 8, 128], BF16) for i in range(2)]
                GTt = [sb(f"GTt{i}", [128, 16, 128], BF16) for i in range(3)]
                HMt = [sb(f"HMt{i}", [128, 512], BF16) for i in range(3)]
                xt = [sb(f"xt{i}", [128, D], F32) for i in range(3)]
                score = sb("score", [128, S], F32)
                work = sb("work", [128, S], BF16)
                rstg = [sb(f"rstg{i}", [128, 512], F32) for i in range(2)]
                m8 = sb("m8", [128, 8], F32)
                mask = sb("mask", [128, S], BF16)
                mTs = [sb(f"mT{i}", [128, 16, 128], BF16) for i in range(2)]
                blo = sb("blo", [128, 1], F32)
                bw0 = sb("bw0", [128, 1], F32)
                bmid = sb("bmid", [128, 1], F32)
                bwt = sb("bwt", [128, NBIS + 1], F32)
                bcnt = sb("bcnt", [128, 1], F32)
                bsw = sb("bsw", [128, 1], F32)
                ME = [sb(f"ME{i}", [128, 8, 128], BF16) for i in range(2)]
                Pb = [[sb(f"Pb{i}{hh}", [128, 512], BF16) for hh in range(2)] for i in range(2)]
                oa = sb("oa", [128, 512], BF16)
                rden = sb("rden", [128, 8], F32)
                osbs = [sb(f"osb{i}", [128, 2, 260], F32) for i in range(2)]
                oaT = sb("oaT", [128, 4, 128], BF16)
                hmT = sb("hmT", [128, 4, 128], BF16)
                t1 = sb("t1", [128, D], F32)
                t2 = sb("t2", [128, D], F32)
                yT = sb("yT", [128, 8, 128], BF16)
                z = sb("z", [128, D], F32)
                x1 = sb("x1", [128, D], F32)
                stats = sb("stats", [128, 12], F32)
                mv = sb("mv", [128, 4], F32)
                x1Tf = sb("x1Tf", [128, 8, 128], F32)
                x1Tb = sb("x1Tb", [128, 8, 128], BF16)
                rt = sb("rt", [128, 64], F32)
                cmbt = sb("cmbt", [128, 16], F32)
                pS = [ps(f"pS{i}", [128, 512], F32) for i in range(2)]
                pTm = ps("pTm", [128, 1024], BF16)
                pL = [ps(f"pL{i}", [128, 512], F32) for i in range(2)]
                pO = [ps(f"pO{i}", [128, 512], F32) for i in range(2)]
                pR = ps("pR", [128, 512], F32)

                def A1g(i):
                    q0 = b0 + i * 128
                    N = 128 * (i + 1)
                    qi, qa, gt, hm, xti = QIt[i % 2], QAt[i % 2], GTt[i % 3], HMt[i % 3], xt[i % 3]
                    tk.dma("sp", qa.t[:], QA[:, :, q0:q0 + 128].rearrange("h d t -> d h t"), writes=[qa])
                    tk.dma("sp", gt.t[:], GT[:, :, q0:q0 + 128].rearrange("m p t -> p m t"), writes=[gt])
                    tk.dma("sp", hm.t[:], HM[q0:q0 + 128, :], writes=[hm])
                    tk.dma("sp", xti.t[:], x[q0:q0 + 128, :], writes=[xti])
                    if i >= 2:
                        nch = (N + 511) // 512
                        n = 0
                        for h in range(8):
                            for cch in range(nch):
                                c0 = cch * 512
                                cwid = min(512, N - c0)
                                p = pS[n % 2]
                                rg = rstg[n % 2]
                                n += 1
                                tk.op("pe", "matmul", p.t[:, :cwid], qi.t[:, h, :], KIs.t[:, c0:c0 + cwid], start=True, stop=True,
                                      reads=[qi, KIs], writes=[p])
                                tk.op("act", "activation", out=rg.t[:, :cwid], in_=p.t[:, :cwid], func=AF.Relu, reads=[p], writes=[rg])
                                if h == 0:
                                    tk.op("dve", "tensor_scalar", score.t[:, c0:c0 + cwid], rg.t[:, :cwid], smb.t[:, i, 0:1], None,
                                          ALU.mult, reads=[rg, smb], writes=[score])
                                else:
                                    tk.op("dve", "scalar_tensor_tensor", score.t[:, c0:c0 + cwid], rg.t[:, :cwid], smb.t[:, i, h:h + 1],
                                          score.t[:, c0:c0 + cwid], ALU.mult, ALU.add, reads=[rg, smb], writes=[score])
                                yield
                        yield "scores_done"
                        tk.op("dve", "tensor_reduce", blo.t[:, :], score.t[:, :N], AX.X, ALU.min, reads=[score], writes=[blo])
                        tk.op("dve", "tensor_tensor", score.t[:, N - 128:N], score.t[:, N - 128:N], trineg.t[:, :], ALU.add,
                              reads=[trineg], writes=[score])
                        tk.op("dve", "tensor_reduce", bw0.t[:, :], score.t[:, :N], AX.X, ALU.max, reads=[score], writes=[bw0])
                        tk.op("dve", "tensor_tensor", bw0.t[:, :], bw0.t[:, :], blo.t[:, :], ALU.subtract, reads=[blo], writes=[bw0])
                        tk.op("dve", "tensor_scalar", bwt.t[:, :], ckc.t[:, :], bw0.t[:, 0:1], None, ALU.mult, reads=[ckc, bw0], writes=[bwt])
                        tk.op("dve", "tensor_tensor", bmid.t[:, :], blo.t[:, :], bwt.t[:, 0:1], ALU.add, reads=[blo, bwt], writes=[bmid])
                        for r in range(NBIS):
                            tk.op("dve", "tensor_scalar", work.t[:, :N], score.t[:, :N], bmid.t[:, 0:1], 0.0, ALU.is_ge, ALU.add,
                                  accum_out=bcnt.t[:, 0:1], reads=[score, bmid], writes=[work, bcnt])
                            tk.op("dve", "tensor_scalar", bsw.t[:, :], bcnt.t[:, :], 255.5, 0.5, ALU.is_ge, ALU.subtract, reads=[bcnt], writes=[bsw])
                            tk.op("dve", "scalar_tensor_tensor", bmid.t[:, :], bsw.t[:, :], bwt.t[:, r:r + 1], bmid.t[:, :], ALU.mult, ALU.add,
                                  reads=[bsw, bwt], writes=[bmid])
                            yield
                        tk.op("dve", "tensor_tensor", blo.t[:, :], bmid.t[:, :], bwt.t[:, NBIS:NBIS + 1], ALU.subtract, reads=[bmid, bwt], writes=[blo])
                        tk.op("dve", "tensor_scalar", mask.t[:, :N], score.t[:, :N], blo.t[:, 0:1], None, ALU.is_ge,
                              reads=[score, blo], writes=[mask])
                    return
                    yield

                def load_qi(i):
                    if 2 <= i < ntile:
                        q0_ = b0 + i * 128
                        tk.dma("sp", QIt[i % 2].t[:], QI[:, :, q0_:q0_ + 128].rearrange("h d t -> d h t"), writes=[QIt[i % 2]])

                def A2(i):
                    mT = mTs[i % 2]
                    if i >= 2:
                        for j0 in range(0, i + 1, 8):
                            nb = min(8, i + 1 - j0)
                            for jj in range(nb):
                                j = j0 + jj
                                tk.op("pe", "transpose", pTm.t[:, jj * 128:(jj + 1) * 128], mask.t[:, j * 128:(j + 1) * 128], identb.t[:, :],
                                      reads=[mask, identb], writes=[pTm])
                            tk.op("act", "copy", mT.t[:, j0:j0 + nb, :], pTm.t[:, :nb * 128].rearrange("p (j t) -> p j t", t=128),
                                  reads=[pTm], writes=[mT])
                    else:
                        for j in range(i):
                            tk.op("pool", "tensor_copy", mT.t[:, j, :], onesb.t[:, :], reads=[onesb], writes=[mT])
                        tk.op("pool", "tensor_copy", mT.t[:, i, :], trib.t[:, :], reads=[trib], writes=[mT])

                def Bg(i):
                    q0 = b0 + i * 128
                    N = 128 * (i + 1)
                    qi, qa, gt, hm, xti = QIt[i % 2], QAt[i % 2], GTt[i % 3], HMt[i % 3], xt[i % 3]
                    mT = mTs[i % 2]
                    tk.op("pool", "tensor_tensor", ME[0].t[:, :, :], erel.t[:, 0, :, :], mT.t[:, i, :].unsqueeze(1).broadcast_to([128, 8, 128]),
                          ALU.mult, reads=[erel, mT], writes=[ME[0]])
                    if i > 0:
                        tk.op("pool", "tensor_tensor", ME[1].t[:, :, :], erel.t[:, 1, :, :], mT.t[:, i - 1, :].unsqueeze(1).broadcast_to([128, 8, 128]),
                              ALU.mult, reads=[erel, mT], writes=[ME[1]])
                    hps = [(j, hh) for j in range(i + 1) for hh in range(2)]

                    def front(j, hh):
                        js = slice(j * 128, (j + 1) * 128)
                        pb = Pb[j % 2]
                        for h4 in range(4):
                            h = hh * 4 + h4
                            tk.op("pe", "matmul", pL[hh].t[:, h4 * 128:(h4 + 1) * 128], KTs[h].t[:, js], qa.t[:, h, :],
                                  start=True, stop=True, reads=[KTs[h], qa], writes=[pL[hh]])
                        tk.op("act", "activation", out=pb[hh].t[:, :], in_=pL[hh].t[:, :], func=AF.Exp, scale=rs.t[:, j:j + 1],
                              reads=[pL[hh], rs], writes=[pb[hh]])
                        pv = pb[hh].t[:, :].rearrange("p (h t) -> p h t", h=4)
                        if j == i:
                            mk_ap, mk_b = ME[0].t[:, hh * 4:(hh + 1) * 4, :], ME[0]
                        elif j == i - 1:
                            mk_ap, mk_b = ME[1].t[:, hh * 4:(hh + 1) * 4, :], ME[1]
                        else:
                            mk_ap, mk_b = mT.t[:, j, :].unsqueeze(1).broadcast_to([128, 4, 128]), mT
                        tk.op("pool", "tensor_tensor", pv, pv, mk_ap, ALU.mult, reads=[mk_b], writes=[pb[hh]])

                    def back(j, hh):
                        pb = Pb[j % 2]
                        for h4 in range(4):
                            h = hh * 4 + h4
                            tk.op("pe", "matmul", pO[hh].t[:, h4 * 65:(h4 + 1) * 65], pb[hh].t[:, h4 * 128:(h4 + 1) * 128],
                                  VAs.t[:, j, h * 65:(h + 1) * 65], start=(j == 0 and h4 == 0), stop=(j == i), skip_group_check=True,
                                  reads=[pb[hh], VAs], writes=[pO[hh]])

                    front(*hps[0])
                    front(*hps[1])
                    for kk in range(len(hps)):
                        if kk + 2 < len(hps):
                            front(*hps[kk + 2])
                        back(*hps[kk])
                        yield
                    osb = osbs[i % 2]
                    for hh in range(2):
                        tk.op("act", "copy", osb.t[:, hh, :], pO[hh].t[:, 0:260], reads=[pO[hh]], writes=[osb])

                def tail(i):
                    q0 = b0 + i * 128
                    gt, hm, xti = GTt[i % 3], HMt[i % 3], xt[i % 3]
                    osb = osbs[i % 2]
                    for hh in range(2):
                        ov = osb.t[:, hh, :].rearrange("p (h d) -> p h d", d=65)
                        tk.op("dve", "reciprocal", rden.t[:, hh * 4:(hh + 1) * 4], ov[:, :, 64], reads=[osb], writes=[rden])
                        tk.op("dve", "tensor_tensor", oa.t[:, hh * 256:(hh + 1) * 256].rearrange("p (h d) -> p h d", d=64), ov[:, :, 0:64],
                              rden.t[:, hh * 4:(hh + 1) * 4].unsqueeze(2).broadcast_to([128, 4, 64]), ALU.mult, reads=[osb, rden], writes=[oa])
                    for k in range(4):
                        tk.op("pe", "transpose", pTm.t[:, k * 128:(k + 1) * 128], oa.t[:, k * 128:(k + 1) * 128], identb.t[:, :],
                              reads=[oa, identb], writes=[pTm])
                    for k in range(4):
                        tk.op("pe", "transpose", pTm.t[:, (4 + k) * 128:(5 + k) * 128], hm.t[:, k * 128:(k + 1) * 128], identb.t[:, :],
                              reads=[hm, identb], writes=[pTm])
                    tk.op("act", "copy", oaT.t[:, :, :], pTm.t[:, 0:512].rearrange("p (k t) -> p k t", t=128), reads=[pTm], writes=[oaT])
                    tk.op("act", "copy", hmT.t[:, :, :], pTm.t[:, 512:1024].rearrange("p (k t) -> p k t", t=128), reads=[pTm], writes=[hmT])

                    for hh in range(2):
                        hs = slice(hh * 512, (hh + 1) * 512)
                        for m4 in range(4):
                            m = hh * 4 + m4
                            for k in range(4):
                                tk.op("pe", "matmul", pR.t[:, m4 * 128:(m4 + 1) * 128], wua.t[:, k, m * 128:(m + 1) * 128], oaT.t[:, k, :],
                                      start=(k == 0), stop=(k == 3), reads=[wua, oaT], writes=[pR])
                        yield
                        tk.op("dve", "tensor_tensor", t1.t[:, hs], pR.t[:, :], gt.t[:, hh * 4:(hh + 1) * 4, :].rearrange("p m t -> p (m t)"),
                              ALU.mult, reads=[pR, gt], writes=[t1])
                        for m4 in range(4):
                            m = hh * 4 + m4
                            for k in range(4):
                                tk.op("pe", "matmul", pR.t[:, m4 * 128:(m4 + 1) * 128], wum.t[:, k, m * 128:(m + 1) * 128], hmT.t[:, k, :],
                                      start=(k == 0), stop=(k == 3), reads=[wum, hmT], writes=[pR])
                        yield
                        tk.op("dve", "tensor_tensor", t2.t[:, hs], pR.t[:, :], gt.t[:, 8 + hh * 4:8 + (hh + 1) * 4, :].rearrange("p m t -> p (m t)"),
                              ALU.mult, reads=[pR, gt], writes=[t2])
                        tk.op("pool", "tensor_tensor", yT.t[:, hh * 4:(hh + 1) * 4, :].rearrange("p m t -> p (m t)"), t1.t[:, hs], t2.t[:, hs],
                              ALU.add, reads=[t1, t2], writes=[yT])
                    for nn in range(2):
                        for k in range(8):
                            tk.op("pe", "matmul", pR.t[:, :], yT.t[:, k, :], wo.t[:, k, nn * 512:(nn + 1) * 512],
                                  start=(k == 0), stop=(k == 7), reads=[yT, wo], writes=[pR])
                        yield
                        ns = slice(nn * 512, (nn + 1) * 512)
                        tk.op("dve", "scalar_tensor_tensor", z.t[:, ns], xti.t[:, ns], ALPHA, pR.t[:, :], ALU.mult, ALU.add,
                              reads=[xti, pR], writes=[z])
                    tk.op("act", "activation", out=t1.t[:, :], in_=z.t[:, :], func=AF.Identity, accum_out=mv.t[:, 0:1], reads=[z], writes=[t1, mv])
                    tk.op("act", "activation", out=t1.t[:, :], in_=z.t[:, :], func=AF.Square, accum_out=mv.t[:, 1:2], reads=[z], writes=[t1, mv])
                    yield
                    tk.op("dve", "tensor_scalar", mv.t[:, 0:1], mv.t[:, 0:1], 1.0 / D, None, ALU.mult, writes=[mv])
                    tk.op("dve", "tensor_tensor", mv.t[:, 3:4], mv.t[:, 0:1], mv.t[:, 0:1], ALU.mult, writes=[mv])
                    tk.op("dve", "scalar_tensor_tensor", mv.t[:, 1:2], mv.t[:, 1:2], 1.0 / D, mv.t[:, 3:4], ALU.mult, ALU.subtract, writes=[mv])
                    tk.op("act", "activation", out=mv.t[:, 2:3], in_=mv.t[:, 1:2], func=AF.Sqrt, bias=epsc.t[:, 0:1], reads=[epsc], writes=[mv])
                    yield
                    tk.op("dve", "reciprocal", mv.t[:, 2:3], mv.t[:, 2:3], writes=[mv])
                    tk.op("dve", "tensor_scalar", x1.t[:, :], z.t[:, :], mv.t[:, 0:1], mv.t[:, 2:3], ALU.subtract, ALU.mult, reads=[z, mv], writes=[x1])
                    tk.op("pool", "tensor_tensor", x1.t[:, :], x1.t[:, :], g1.t[:, :], ALU.mult, reads=[g1], writes=[x1])
                    tk.op("pool", "tensor_tensor", x1.t[:, :], x1.t[:, :], b1.t[:, :], ALU.add, reads=[b1], writes=[x1])
                    tk.dma("sp", X1[q0:q0 + 128, :], x1.t[:, :], reads=[x1])
                    for hh in range(2):
                        for k4 in range(4):
                            k = hh * 4 + k4
                            tk.op("pe", "matmul", pR.t[:, k4 * 128:(k4 + 1) * 128], x1.t[:, k * 128:(k + 1) * 128], identf.t[:, :],
                                  start=True, stop=True, reads=[x1, identf], writes=[pR])
                        tk.op("act", "copy", x1Tf.t[:, hh * 4:(hh + 1) * 4, :].rearrange("p k t -> p (k t)"), pR.t[:, :], reads=[pR], writes=[x1Tf])
                        tk.op("act", "copy", x1Tb.t[:, hh * 4:(hh + 1) * 4, :].rearrange("p k t -> p (k t)"), pR.t[:, :], reads=[pR], writes=[x1Tb])
                        yield
                    tk.dma("sp", X1T[:, :, q0:q0 + 128].rearrange("k p t -> p k t"), x1Tb.t[:, :, :], reads=[x1Tb])
                    for k in range(8):
                        tk.op("pe", "matmul", pR.t[:, 0:20], x1Tf.t[:, k, :], wrs.t[:, k, :], start=(k == 0), stop=(k == 7),
                              reads=[x1Tf, wrs], writes=[pR])
                    yield
                    L = rt.t
                    W = dict(reads=[], writes=[rt])
                    tk.op("dve", "tensor_tensor", L[:, 0:20], pR.t[:, 0:20], brs.t[:, :], ALU.add, reads=[pR, brs], writes=[rt])
                    tk.op("dve", "reduce_max", L[:, 20:21], L[:, 0:4], AX.X, **W)
                    tk.op("dve", "tensor_scalar", L[:, 24:28], L[:, 0:4], L[:, 20:21], None, ALU.is_ge, **W)
                    tk.op("dve", "tensor_scalar", L[:, 28:32], L[:, 0:4], L[:, 20:21], None, ALU.subtract, **W)
                    tk.op("act", "activation", out=L[:, 28:32], in_=L[:, 28:32], func=AF.Exp, accum_out=L[:, 21:22], **W)
                    tk.op("dve", "tensor_tensor", L[:, 32:48].rearrange("p (g e) -> p g e", e=4), L[:, 4:20].rearrange("p (g e) -> p g e", e=4),
                          L[:, 24:28].unsqueeze(2).broadcast_to([128, 4, 4]), ALU.mult, **W)
                    tk.op("dve", "tensor_reduce", L[:, 48:52], L[:, 32:48].rearrange("p (g e) -> p e g", e=4), AX.X, ALU.add, **W)
                    tk.op("dve", "reduce_max", L[:, 52:53], L[:, 48:52], AX.X, **W)
                    tk.op("dve", "tensor_scalar", L[:, 56:60], L[:, 48:52], L[:, 52:53], None, ALU.is_ge, **W)
                    tk.op("dve", "scalar_tensor_tensor", L[:, 60:64], L[:, 56:60], NEG, L[:, 48:52], ALU.mult, ALU.add, **W)
                    tk.op("dve", "reduce_max", L[:, 53:54], L[:, 60:64], AX.X, **W)
                    tk.op("dve", "tensor_scalar", L[:, 60:64], L[:, 60:64], L[:, 53:54], None, ALU.is_ge, **W)
                    tk.op("dve", "tensor_tensor", L[:, 54:55], L[:, 53:54], L[:, 52:53], ALU.subtract, **W)
                    tk.op("act", "activation", out=L[:, 55:56], in_=L[:, 54:55], func=AF.Sigmoid, scale=-1.0, **W)
                    yield
                    tk.op("dve", "reciprocal", L[:, 22:23], L[:, 21:22], **W)
                    tk.op("dve", "tensor_tensor", L[:, 55:56], L[:, 55:56], L[:, 22:23], ALU.mult, **W)
                    tk.op("dve", "tensor_tensor", L[:, 54:55], L[:, 22:23], L[:, 55:56], ALU.subtract, **W)
                    tk.op("dve", "tensor_scalar", L[:, 56:60], L[:, 56:60], L[:, 55:56], None, ALU.mult, **W)
                    tk.op("dve", "scalar_tensor_tensor", L[:, 56:60], L[:, 60:64], L[:, 54:55], L[:, 56:60], ALU.mult, ALU.add, **W)
                    tk.op("dve", "tensor_tensor", cmbt.t[:, :].rearrange("p (g e) -> p g e", e=4),
                          L[:, 24:28].unsqueeze(2).broadcast_to([128, 4, 4]), L[:, 56:60].unsqueeze(1).broadcast_to([128, 4, 4]),
                          ALU.mult, reads=[rt], writes=[cmbt])
                    tk.dma("sp", CMB[q0:q0 + 128, :], cmbt.t[:, :], reads=[cmbt])

                def adv(g, n=1):
                    if g is None:
                        return
                    for _ in range(n):
                        if next(g, "done") == "done":
                            return

                def drain(g):
                    if g is not None:
                        for _ in g:
                            pass

                def sched(a1, others, nscore):
                    others = [o for o in others if o is not None]
                    if a1 is None:
                        for g, _ in reversed(others):
                            drain(g)
                        return
                    total = SCW * nscore + NBIS
                    credit = [0.0] * len(others)
                    done = [False] * len(others)
                    wgt = SCW
                    while True:
                        v = next(a1, "done")
                        if v == "done":
                            break
                        if v == "scores_done":
                            wgt = 1.0
                            continue
                        for oi, (g, nsteps) in enumerate(others):
                            if done[oi]:
                                continue
                            credit[oi] += (nsteps + 1) * wgt / total
                            while credit[oi] >= 1.0 and not done[oi]:
                                credit[oi] -= 1.0
                                if next(g, "done") == "done":
                                    done[oi] = True
                    for g, _ in others:
                        drain(g)

                load_qi(2)
                drain(A1g(0))
                A2(0)
                for i in range(ntile):
                    if i >= 1:
                        load_qi(i + 2)
                    a1 = A1g(i + 1) if i + 1 < ntile else None
                    if a1 is not None and i + 1 < 2:
                        drain(a1)
                        a1 = None
                    sched(a1, [(Bg(i), 2 * (i + 1)), (tail(i - 1), 13) if i >= 1 else None], 8 * ((128 * (i + 2) + 511) // 512))
                    if i + 1 < ntile:
                        A2(i + 1)
                drain(tail(ntile - 1))
            s34.close()
            tk.barrier()
            if stop < 5:
                tk.finish()
                return nc

        with ExitStack() as s5:
            sb, ps = mk(s5)
            x1T = [sb(f"x1T{k}", [128, S], BF16) for k in range(8)]
            x1Tg = [[Buf(x1T[k].t[:, g * 512:(g + 1) * 512]) for g in range(4)] for k in range(8)]
            acc = [sb(f"acc{t_}", [128, D], F32) for t_ in range(16)]
            cmbs = [sb(f"cmb{i}", [128, 16, 16], F32) for i in range(2)]
            eps2 = sb("eps2", [128, 1], F32)
            tk.op("dve", "memset", eps2.t[:], LN_EPS / (ALPHA * ALPHA), writes=[eps2])
            g2 = sb("g2", [128, D], F32)
            b2 = sb("b2", [128, D], F32)
            wg = [sb(f"wg{i}", [128, 8, 512], BF16) for i in range(2)]
            wu = [sb(f"wu{i}", [128, 8, 512], BF16) for i in range(2)]
            wd = [sb(f"wd{i}", [128, 4, D], BF16) for i in range(2)]
            hT = [sb(f"hT{i}", [128, 4, 512], BF16) for i in range(2)]
            sg = [sb(f"sg{i}", [128, 512], F32) for i in range(2)]
            ot = [sb(f"ot{i}", [128, D], F32) for i in range(2)]
            mv = sb("mv5", [128, 4], F32)
            pG = [ps(f"pG{i}", [128, 512], F32) for i in range(2)]
            pU = [ps(f"pU{i}", [128, 512], F32) for i in range(2)]
            pD = [ps(f"pD{i}", [128, 512], F32) for i in range(4)]

            def load_x1T(b, grp):
                c0 = b * S + grp * 512
                for k in range(8):
                    tk.dma("sp", x1Tg[k][grp].t, X1T[k, :, c0:c0 + 512], writes=[x1Tg[k][grp]])

            def load_acc(b, t_):
                r0 = b * S + t_ * 128
                tk.dma("sp", acc[t_].t[:], X1[r0:r0 + 128, :], writes=[acc[t_]])

            def load_cmb(b):
                cm = cmbs[b]
                tk.dma("sp", cm.t[:], CMB[b * S:(b + 1) * S, :].rearrange("(c p) e -> p c e", p=128), writes=[cm])
                tk.op("dve", "tensor_scalar", cm.t[:, :, :], cm.t[:, :, :], 1.0 / ALPHA, None, ALU.mult, writes=[cm])

            def loadw(ge):
                e, i2 = ge % 16, ge % 2
                tk.dma("pool", wg[i2].t[:], w_gate[e].rearrange("(k p) f -> p k f", p=128), writes=[wg[i2]])
                tk.dma("pool", wu[i2].t[:], w_up[e].rearrange("(k p) f -> p k f", p=128), writes=[wu[i2]])
                tk.dma("pool", wd[i2].t[:], w_down[e].rearrange("(k p) f -> p k f", p=128), writes=[wd[i2]])

            def ln2(b, tt):
                a = acc[tt]
                o = ot[tt % 2]
                r0 = b * S + tt * 128
                tk.op("act", "activation", out=o.t[:, :], in_=a.t[:, :], func=AF.Identity, accum_out=mv.t[:, 0:1], reads=[a], writes=[o, mv])
                tk.op("act", "activation", out=o.t[:, :], in_=a.t[:, :], func=AF.Square, accum_out=mv.t[:, 1:2], reads=[a], writes=[o, mv])
                tk.op("dve", "tensor_scalar", mv.t[:, 0:1], mv.t[:, 0:1], 1.0 / D, None, ALU.mult, writes=[mv])
                tk.op("dve", "tensor_tensor", mv.t[:, 3:4], mv.t[:, 0:1], mv.t[:, 0:1], ALU.mult, writes=[mv])
                tk.op("dve", "scalar_tensor_tensor", mv.t[:, 1:2], mv.t[:, 1:2], 1.0 / D, mv.t[:, 3:4], ALU.mult, ALU.subtract, writes=[mv])
                tk.op("act", "activation", out=mv.t[:, 2:3], in_=mv.t[:, 1:2], func=AF.Sqrt, bias=eps2.t[:, 0:1], reads=[eps2], writes=[mv])
                tk.op("dve", "reciprocal", mv.t[:, 2:3], mv.t[:, 2:3], writes=[mv])
                tk.op("dve", "tensor_scalar", o.t[:, :], a.t[:, :], mv.t[:, 0:1], mv.t[:, 2:3], ALU.subtract, ALU.mult, reads=[a, mv], writes=[o])
                tk.op("pool", "tensor_tensor", o.t[:, :], o.t[:, :], g2.t[:, :], ALU.mult, reads=[g2], writes=[o])
                tk.op("pool", "tensor_tensor", o.t[:, :], o.t[:, :], b2.t[:, :], ALU.add, reads=[b2], writes=[o])
                tk.dma("sp", out[r0:r0 + 128, :], o.t[:, :], reads=[o])

            loadw(0)
            for grp in range(4):
                load_x1T(0, grp)
            load_cmb(0)
            for t_ in range(16):
                load_acc(0, t_)
            tk.dma("sp", g2.t[:], ln2g, writes=[g2])
            tk.dma("sp", b2.t[:], ln2b, writes=[b2])
            n = 0
            nd = 0
            for b in range(2):
                cmb = cmbs[b]
                for e in range(16):
                    ge = b * 16 + e
                    last = (e == 15)
                    if ge + 1 < 32:
                        loadw(ge + 1)
                    if last and b == 0:
                        load_cmb(1)
                    i2 = ge % 2
                    for grp in range(4):
                        gs = slice(grp * 512, (grp + 1) * 512)
                        hb = hT[grp % 2]
                        for f in range(4):
                            pg_, pu_, sg_ = pG[n % 2], pU[n % 2], sg[n % 2]
                            n += 1
                            for k in range(8):
                                tk.op("pe", "matmul", pg_.t[:, :], wg[i2].t[:, k, f * 128:(f + 1) * 128], x1T[k].t[:, gs],
                                      start=(k == 0), stop=(k == 7), reads=[wg[i2], x1Tg[k][grp]], writes=[pg_])
                            for k in range(8):
                                tk.op("pe", "matmul", pu_.t[:, :], wu[i2].t[:, k, f * 128:(f + 1) * 128], x1T[k].t[:, gs],
                                      start=(k == 0), stop=(k == 7), reads=[wu[i2], x1Tg[k][grp]], writes=[pu_])
                            tk.op("act", "activation", out=sg_.t[:, :], in_=pg_.t[:, :], func=AF.Silu, reads=[pg_], writes=[sg_])
                            tk.op("dve", "tensor_tensor", hb.t[:, f, :], sg_.t[:, :], pu_.t[:, :], ALU.mult, reads=[sg_, pu_], writes=[hb])
                        if last and b == 0:
                            load_x1T(1, grp)
                        for j in range(4):
                            tt = grp * 4 + j
                            for nn in range(2):
                                pd_ = pD[nd % 4]
                                nd += 1
                                for f in range(4):
                                    tk.op("pe", "matmul", pd_.t[:, :], hb.t[:, f, j * 128:(j + 1) * 128], wd[i2].t[:, f, nn * 512:(nn + 1) * 512],
                                          start=(f == 0), stop=(f == 3), reads=[hb, wd[i2]], writes=[pd_])
                                ns = slice(nn * 512, (nn + 1) * 512)
                                tk.op("dve", "scalar_tensor_tensor", acc[tt].t[:, ns], pd_.t[:, :], cmb.t[:, tt, e:e + 1], acc[tt].t[:, ns],
                                      ALU.mult, ALU.add, reads=[pd_, cmb], writes=[acc[tt]])
                            if last:
                                ln2(b, tt)
                                if b == 0:
                                    load_acc(1, tt)
        tk.barrier()
        tk.finish()
    return nc


def _t5_bucket_np(d):
    d = np.maximum(d, 0)
    ratio = np.log(np.maximum(d, 1).astype(np.float32) / np.float32(16)) / np.float32(math.log(128 / 16))
    large = np.minimum(16 + (ratio * 16).astype(np.int32), 31)
    return np.where(d < 16, d, large)


def _consts():
    idx = np.arange(128)
    ident = np.eye(128, dtype=np.float32)
    tri = (idx[:, None] <= idx[None, :]).astype(np.float32)
    trineg = np.where(idx[None, :] <= idx[:, None], 0.0, NEG).astype(np.float32)
    ones = np.ones((128, 128), np.float32)
    oh = np.zeros((32, 2, 128, 128), np.float32)
    for blk in range(2):
        dd = idx[:, None] - idx[None, :] + 128 * blk
        bk = _t5_bucket_np(dd)
        for bb in range(32):
            oh[bb, blk] = (bk == bb)
    return ident, tri, trineg, ones, oh


def kernel(x, w_in, conv_w, conv_b, kv_norm_g, w_uk, w_uv, rel_bias, b_i, b_f, mh_norm_g, w_up_a, w_up_m, w_out,
           ln1_g, ln1_b, w_grp, b_grp, w_rt, b_rt, w_gate, w_up, w_down, ln2_g, ln2_b):
    f = lambda a: np.ascontiguousarray(np.asarray(a, dtype=np.float32))
    x = f(x)
    rep = lambda v, n: f(np.broadcast_to(np.asarray(v, np.float32).reshape(1, n), (128, n)))
    ident, tri, trineg, ones, oh = _consts()
    rel_bias = f(rel_bias)
    shared = {
        "w_in": f(w_in[0]),
        "conv_w": f(np.asarray(conv_w[0]).T.reshape(8, 128, 4).transpose(1, 0, 2)),
        "conv_b": f(np.asarray(conv_b[0]).reshape(8, 128, 1).transpose(1, 0, 2)),
        "kvg": f(np.asarray(kv_norm_g[0]).reshape(2, 128, 1).transpose(1, 0, 2)),
        "w_uk": f(np.asarray(w_uk[0]).reshape(256, 512)),
        "w_uv": f(np.asarray(w_uv[0]).reshape(256, 512)),
        "relb": rel_bias,
        "relb31": f(np.broadcast_to(rel_bias[31:32, :], (32, 8))),
        "oh": oh,
        "bif": rep(np.concatenate([np.asarray(b_i[0]), np.asarray(b_f[0])]), 8),
        "mhg": rep(np.asarray(mh_norm_g[0]).reshape(-1), 512),
        "w_up_a": f(w_up_a[0]), "w_up_m": f(w_up_m[0]), "w_out": f(w_out[0]),
        "ln1g": rep(ln1_g[0], D), "ln1b": rep(ln1_b[0], D), "ln2g": rep(ln2_g[0], D), "ln2b": rep(ln2_b[0], D),
        "w_r": f(np.concatenate([np.asarray(w_grp[0]), np.asarray(w_rt[0])], axis=1)),
        "b_r": rep(np.concatenate([np.asarray(b_grp[0]), np.asarray(b_rt[0])]), 20),
        "w_gate": f(w_gate[0]), "w_up": f(w_up[0]), "w_down": f(w_down[0]),
        "c_ident": ident, "c_tri": tri, "c_trineg": trineg, "c_ones": ones,
        "c_ck": np.ascontiguousarray(np.broadcast_to((0.5 ** np.arange(1, NBIS + 2, dtype=np.float64)).astype(np.float32)[None, :], (128, NBIS + 1))),
    }
    if _DBG.get("on"):
        return shared, x
    nc = build_nc()
    in_maps = []
    for c in range(NCORES):
        m = dict(shared)
        m["x"] = np.ascontiguousarray(x[2 * c:2 * c + 2].reshape(T, D))
        in_maps.append(m)
    res = run_bass_kernel_spmd(nc, in_maps, core_ids=list(range(NCORES)))
    outs = [np.asarray(r["out"], dtype=np.float32).reshape(2, S, D) for r in res.results]
    return np.concatenate(outs, axis=0)
```

```python
import math
import os
from contextlib import ExitStack

import numpy as np
import concourse.bass as bass
import concourse.mybir as mybir
from concourse.bass_utils import run_bass_kernel_spmd

F32 = mybir.dt.float32
BF16 = mybir.dt.bfloat16
AF = mybir.ActivationFunctionType
ALU = mybir.AluOpType
AX = mybir.AxisListType

NCORES = 8
_DBG = {}
T = 4096
S = 2048
D = 1024
D_IN = 5456
ALPHA = 2.0 ** 0.25
LN_EPS = 1e-5
NEG = -1.0e30
NBIS = 20
SCW = 0.0
C_QA, C_CKV, C_QI, C_KI, C_WI, C_QKM, C_VM, C_IM, C_FM, C_OM, C_GA, C_GM = (
    0, 512, 768, 1280, 1344, 1352, 2376, 2888, 2892, 2896, 3408, 4432)


class Buf:
    __slots__ = ("t", "w", "r")

    def __init__(self, t):
        self.t = t
        self.w = {}
        self.r = {}


class TK:
    def __init__(self, nc, es):
        self.nc = nc
        self.E = {"pe": nc.tensor, "act": nc.scalar, "dve": nc.vector, "pool": nc.gpsimd, "sp": nc.sync}
        self.sem = {}
        self.cnt = {}
        for e in ("pe", "act", "dve", "pool"):
            self.sem[e] = es.enter_context(nc.semaphore("s_" + e))
            self.cnt[e] = 0
        self.dq = {"sp": [], "pool": []}
        self.dqi = {"sp": 0, "pool": 0}
        for q, n in (("sp", 8), ("pool", 6)):
            for i in range(n):
                nm = f"d_{q}{i}"
                self.sem[nm] = es.enter_context(nc.semaphore(nm))
                self.cnt[nm] = 0
                self.dq[q].append(nm)
        self.seen = {e: {} for e in self.E}

    def wait(self, e, deps):
        for s, v in deps.items():
            if e == "pe" and s == "pe":
                continue
            if self.seen[e].get(s, 0) >= v:
                continue
            self.E[e].wait_ge(self.sem[s], v)
            self.seen[e][s] = v

    @staticmethod
    def _merge(d, src):
        for s, v in src.items():
            if d.get(s, 0) < v:
                d[s] = v

    def _deps(self, reads, writes):
        deps = {}
        for b in reads:
            self._merge(deps, b.w)
        for b in writes:
            self._merge(deps, b.w)
            self._merge(deps, b.r)
        return deps

    def _post(self, s, v, reads, writes):
        for b in reads:
            b.r[s] = v
        for b in writes:
            b.w = {s: v}
            b.r = {}

    def op(self, e, method, *args, reads=(), writes=(), **kw):
        self.wait(e, self._deps(reads, writes))
        ins = getattr(self.E[e], method)(*args, **kw)
        self.cnt[e] += 1
        ins.then_inc(self.sem[e], 1)
        self._post(e, self.cnt[e], reads, writes)

    def dma(self, q, out, in_, reads=(), writes=()):
        nm = self.dq[q][self.dqi[q] % len(self.dq[q])]
        self.dqi[q] += 1
        deps = self._deps(reads, writes)
        if self.cnt[nm] > 0:
            deps[nm] = max(deps.get(nm, 0), self.cnt[nm])
        self.wait(q, deps)
        self.E[q].dma_start(out=out, in_=in_).then_inc(self.sem[nm], 16)
        self.cnt[nm] += 16
        self._post(nm, self.cnt[nm], reads, writes)

    def barrier(self):
        allv = {s: v for s, v in self.cnt.items() if v > 0}
        for e in self.E:
            self.wait(e, dict(allv))

    def finish(self):
        allv = {s: v for s, v in self.cnt.items() if v > 0 and s.startswith("d_")}
        self.wait("sp", allv)


def build_nc(stop=99, debug=False, ntile=16, sub=99, ngroups=8, nchunks=16):
    nc = bass.Bass("TRN2", target_bir_lowering=False)

    def din(name, shape, dt=F32):
        return nc.dram_tensor(name, shape, dt, kind="ExternalInput").ap()

    def dscr(name, shape, dt):
        return nc.dram_tensor(name, shape, dt, kind="ExternalOutput" if debug else "Internal").ap()

    x = din("x", [T, D])
    w_in = din("w_in", [D, D_IN])
    conv_w = din("conv_w", [128, 8, 4])
    conv_b = din("conv_b", [128, 8, 1])
    kvg = din("kvg", [128, 2, 1])
    w_uk = din("w_uk", [256, 512])
    w_uv = din("w_uv", [256, 512])
    relb = din("relb", [32, 8])
    relb31 = din("relb31", [32, 8])
    oh = din("oh", [32, 2, 128, 128])
    bif = din("bif", [128, 8])
    mhg = din("mhg", [128, 512])
    w_up_a = din("w_up_a", [512, D])
    w_up_m = din("w_up_m", [512, D])
    w_out = din("w_out", [D, D])
    ln1g = din("ln1g", [128, D])
    ln1b = din("ln1b", [128, D])
    ln2g = din("ln2g", [128, D])
    ln2b = din("ln2b", [128, D])
    w_r = din("w_r", [D, 20])
    b_r = din("b_r", [128, 20])
    w_gate = din("w_gate", [16, D, 512])
    w_up = din("w_up", [16, D, 512])
    w_down = din("w_down", [16, 512, D])
    c_ident = din("c_ident", [128, 128])
    c_tri = din("c_tri", [128, 128])
    c_trineg = din("c_trineg", [128, 128])
    c_ones = din("c_ones", [128, 128])
    c_ck = din("c_ck", [128, NBIS + 1])
    out = nc.dram_tensor("out", [T, D], F32, kind="ExternalOutput").ap()

    QA = dscr("QA", [8, 64, T], BF16)
    QI = dscr("QI", [8, 64, T], BF16)
    KI = dscr("KI", [64, T], BF16)
    KT = dscr("KT", [8, 64, T], BF16)
    QKM = dscr("QKM", [8, 128, T], BF16)
    GT = dscr("GT", [16, 128, T], BF16)
    VA = dscr("VA", [T, 520], BF16)
    VM = dscr("VM", [T, 516], BF16)
    OM = dscr("OM", [T, 512], BF16)
    SM = dscr("SM", [T, 32], F32)
    HM = dscr("HM", [T, 512], BF16)
    X1 = dscr("X1", [T, D], F32)
    X1T = dscr("X1T", [8, 128, T], BF16)
    CMB = dscr("CMB", [T, 16], F32)

    with ExitStack() as es:
        tk = TK(nc, es)

        uid = [0]

        def mk(stack):
            def sb(name, shape, dt):
                uid[0] += 1
                return Buf(stack.enter_context(nc.sbuf_tensor(f"{name}_{uid[0]}", shape, dt)))

            def ps(name, shape, dt):
                uid[0] += 1
                return Buf(stack.enter_context(nc.psum_tensor(f"{name}_{uid[0]}", shape, dt)))
            return sb, ps

        gsb, _ = mk(es)
        identf = gsb("identf", [128, 128], F32)
        identb = gsb("identb", [128, 128], BF16)
        trif = gsb("trif", [128, 128], F32)
        trib = gsb("trib", [128, 128], BF16)
        trineg = gsb("trineg", [128, 128], F32)
        onesf = gsb("onesf", [128, 128], F32)
        onesb = gsb("onesb", [128, 128], BF16)
        epsc = gsb("epsc", [128, 1], F32)
        erel = gsb("erel", [128, 2, 8, 128], BF16)
        tk.dma("sp", identf.t[:], c_ident, writes=[identf])
        tk.dma("pool", identb.t[:], c_ident, writes=[identb])
        tk.dma("sp", trif.t[:], c_tri, writes=[trif])
        tk.dma("pool", trib.t[:], c_tri, writes=[trib])
        tk.dma("sp", trineg.t[:], c_trineg, writes=[trineg])
        tk.dma("sp", onesf.t[:], c_ones, writes=[onesf])
        tk.dma("pool", onesb.t[:], c_ones, writes=[onesb])
        tk.op("dve", "memset", epsc.t[:], LN_EPS, writes=[epsc])
        ckc = gsb("ckc", [128, NBIS + 1], F32)
        tk.dma("sp", ckc.t[:], c_ck, writes=[ckc])

        with ExitStack() as s0:
            sb, ps = mk(s0)
            rb = sb("rb", [32, 8], F32)
            rb31 = sb("rb31", [32, 8], F32)
            rba = sb("rba", [32, 8], F32)
            tk.dma("sp", rb.t[:], relb, writes=[rb])
            tk.dma("sp", rb31.t[:], relb31, writes=[rb31])
            tk.op("dve", "tensor_tensor", rba.t[:], rb.t[:], rb31.t[:], ALU.subtract, reads=[rb, rb31], writes=[rba])
            ohc = [sb(f"ohc{i}", [32, 32, 128], F32) for i in range(2)]
            pe_ = [ps(f"pe{i}", [128, 256], F32) for i in range(2)]
            n = 0
            for blk in range(2):
                for tc_ in range(4):
                    o = ohc[n % 2]
                    p = pe_[n % 2]
                    n += 1
                    tk.dma("sp", o.t[:], oh[:, blk, tc_ * 32:(tc_ + 1) * 32, :], writes=[o])
                    for tl in range(32):
                        tk.op("pe", "matmul", p.t[:, tl * 8:(tl + 1) * 8], o.t[:, tl, :], rba.t[:, :],
                              start=True, stop=True, reads=[o, rba], writes=[p])
                    tk.op("act", "activation", out=erel.t[:, blk, :, tc_ * 32:(tc_ + 1) * 32],
                          in_=p.t[:, :].rearrange("p (t h) -> p h t", h=8), func=AF.Exp,
                          reads=[p], writes=[erel])
        tk.barrier()

        with ExitStack() as s1:
            if stop < 1:
                tk.finish()
                return nc
            sb, ps = mk(s1)
            win = [sb(f"win{k}", [128, D_IN], BF16) for k in range(8)]
            xb = [sb(f"xb{i}", [128, 4, D], BF16) for i in range(2)]

            def load_x(g):
                tk.dma("pool", xb[g % 2].t[:], x[g * 512:(g + 1) * 512, :].rearrange("(j p) c -> p j c", p=128), writes=[xb[g % 2]])

            load_x(0)
            SEGS = [C_QA, C_CKV, C_QI, C_KI, C_WI, C_QKM, C_VM, C_IM, C_OM, C_GA, C_GM, D_IN]
            wseg = [[Buf(win[k].t[:, SEGS[si]:SEGS[si + 1]]) for si in range(len(SEGS) - 1)] for k in range(8)]
            for si in (0, 2, 3, 1, 5, 9, 10, 4, 7, 6, 8):
                for k in range(8):
                    tk.dma("pool", wseg[k][si].t, w_in[k * 128:(k + 1) * 128, SEGS[si]:SEGS[si + 1]], writes=[wseg[k][si]])

            def wsl(k, c0, n):
                for si in range(len(SEGS) - 1):
                    if SEGS[si] <= c0 and c0 + n <= SEGS[si + 1]:
                        return win[k].t[:, c0:c0 + n], wseg[k][si]
                raise AssertionError((c0, n))
            cw = sb("cw", [128, 8, 4], F32)
            cb = sb("cb", [128, 8, 1], F32)
            tk.dma("sp", cw.t[:], conv_w, writes=[cw])
            tk.dma("sp", cb.t[:], conv_b, writes=[cb])
            kg = sb("kg", [128, 2, 1], F32)
            tk.dma("sp", kg.t[:], kvg, writes=[kg])
            wkf = sb("wkf", [128, 2, 512], F32)
            wvf = sb("wvf", [128, 2, 512], F32)
            tk.dma("sp", wkf.t[:], w_uk.rearrange("(m p) c -> p m c", p=128), writes=[wkf])
            tk.dma("sp", wvf.t[:], w_uv.rearrange("(m p) c -> p m c", p=128), writes=[wvf])
            wukg = sb("wukg", [128, 2, 512], BF16)
            wuvg = sb("wuvg", [128, 2, 512], BF16)
            for rc in range(2):
                tk.op("dve", "tensor_scalar", wukg.t[:, rc, :], wkf.t[:, rc, :], kg.t[:, rc, 0:1], None, ALU.mult,
                      reads=[wkf, kg], writes=[wukg])
                tk.op("dve", "tensor_scalar", wuvg.t[:, rc, :], wvf.t[:, rc, :], kg.t[:, rc, 0:1], None, ALU.mult,
                      reads=[wvf, kg], writes=[wuvg])
            bifs = sb("bifs", [128, 8], F32)
            tk.dma("sp", bifs.t[:], bif, writes=[bifs])

            xT = [sb(f"xT{i}", [128, 8, 512], BF16) for i in range(2)]
            crT = [sb(f"crT{i}", [128, 2, 512], BF16) for i in range(2)]
            U = [sb(f"U{m}", [128, 515], F32) for m in range(8)]
            cacc = [sb(f"cacc{i}", [128, 512], F32) for i in range(2)]
            stg = [sb(f"stg{i}", [128, 512], BF16) for i in range(10)]
            vms = [sb(f"vms{i}", [128, 4, 129], BF16) for i in range(4)]
            vas = [sb(f"vas{i}", [128, 8, 65], BF16) for i in range(4)]
            sms = [sb(f"sms{i}", [128, 32], F32) for i in range(4)]
            junk = sb("junk", [128, 256], F32)
            ssq = sb("ssq", [128, 1], F32)
            ftmp = sb("ftmp", [128, 4], F32)
            pT = [ps(f"pT{i}", [128, 512], BF16) for i in range(2)]
            pp = [ps(f"pp{i}", [128, 512], F32) for i in range(6)]
            for v in vms:
                tk.op("pool", "memset", v.t[:], 1.0, writes=[v])
            for v in vas:
                tk.op("pool", "memset", v.t[:], 1.0, writes=[v])
            for s_ in sms:
                tk.op("pool", "memset", s_.t[:], 0.0, writes=[s_])
            cnt = {"pp": 0, "stg": 0, "ev": 0}

            def nextpp():
                cnt["pp"] += 1
                return pp[cnt["pp"] % 6]

            def nextstg():
                cnt["stg"] += 1
                return stg[cnt["stg"] % 10]

            def evac_copy(dst_ap, src_ap, reads, writes):
                cnt["ev"] += 1
                if cnt["ev"] % 2:
                    tk.op("act", "copy", dst_ap, src_ap, reads=reads, writes=writes)
                else:
                    tk.op("dve", "tensor_copy", dst_ap, src_ap, reads=reads, writes=writes)

            for g in range(ngroups):
                t0 = g * 512
                xbuf = xb[g % 2]
                xt_ = xT[g % 2]
                cr = crT[g % 2]
                for k in range(8):
                    pt = pT[k % 2]
                    for j in range(4):
                        tk.op("pe", "transpose", pt.t[:, j * 128:(j + 1) * 128], xbuf.t[:, j, k * 128:(k + 1) * 128],
                              identb.t[:], reads=[xbuf, identb], writes=[pt])
                    evac_copy(xt_.t[:, k, :], pt.t[:, :], [pt], [xt_])
                if g + 1 < ngroups:
                    load_x(g + 1)

                def fm(c0, M):
                    p = nextpp()
                    for k in range(8):
                        wap, wbuf = wsl(k, c0, M)
                        tk.op("pe", "matmul", p.t[:M, :], wap, xt_.t[:, k, :],
                              start=(k == 0), stop=(k == 7), reads=[wbuf, xt_], writes=[p])
                    return p

                for h in range(8):
                    p = fm(C_QA + h * 64, 64)
                    st = nextstg()
                    evac_copy(st.t[:64, :], p.t[:64, :], [p], [st])
                    tk.dma("sp", QA[h, :, t0:t0 + 512], st.t[:64, :], reads=[st])
                for h in range(8):
                    p = fm(C_QI + h * 64, 64)
                    st = nextstg()
                    evac_copy(st.t[:64, :], p.t[:64, :], [p], [st])
                    tk.dma("sp", QI[h, :, t0:t0 + 512], st.t[:64, :], reads=[st])
                p = fm(C_KI, 64)
                st = nextstg()
                evac_copy(st.t[:64, :], p.t[:64, :], [p], [st])
                tk.dma("sp", KI[:, t0:t0 + 512], st.t[:64, :], reads=[st])
                for rc in range(2):
                    p = fm(C_CKV + rc * 128, 128)
                    evac_copy(cr.t[:, rc, :], p.t[:, :], [p], [cr])
                for h in range(8):
                    p = nextpp()
                    for rc in range(2):
                        tk.op("pe", "matmul", p.t[:64, :], wukg.t[:, rc, h * 64:(h + 1) * 64], cr.t[:, rc, :],
                              start=(rc == 0), stop=(rc == 1), reads=[wukg, cr], writes=[p])
                    st = nextstg()
                    evac_copy(st.t[:64, :], p.t[:64, :], [p], [st])
                    tk.dma("sp", KT[h, :, t0:t0 + 512], st.t[:64, :], reads=[st])
                for m in range(8):
                    p = fm(C_QKM + m * 128, 128)
                    u = U[m]
                    if g % 4 == 0:
                        tk.op("dve", "memset", u.t[:, 0:3], 0.0, writes=[u])
                    tk.op("act", "copy", u.t[:, 3:515], p.t[:, :], reads=[p], writes=[u])
                    ca = cacc[m % 2]
                    tk.op("dve", "tensor_scalar", ca.t[:, :], u.t[:, 0:512], cw.t[:, m, 0:1], None, ALU.mult,
                          reads=[u, cw], writes=[ca])
                    for j in range(1, 4):
                        tk.op("dve", "scalar_tensor_tensor", ca.t[:, :], u.t[:, j:j + 512], cw.t[:, m, j:j + 1], ca.t[:, :],
                              ALU.mult, ALU.add, reads=[u, cw], writes=[ca])
                    st = nextstg()
                    tk.op("act", "activation", out=st.t[:, :], in_=ca.t[:, :], func=AF.Silu, bias=cb.t[:, m, 0:1],
                          reads=[ca, cb], writes=[st])
                    tk.dma("sp", QKM[m, :, t0:t0 + 512], st.t[:, :], reads=[st])
                    tk.op("dve", "tensor_copy", u.t[:, 0:3], u.t[:, 512:515], writes=[u])
                for m in range(16):
                    p = fm(C_GA + m * 128, 128)
                    st = nextstg()
                    tk.op("act", "activation", out=st.t[:, :], in_=p.t[:, :], func=AF.Sigmoid, reads=[p], writes=[st])
                    tk.dma("sp", GT[m, :, t0:t0 + 512], st.t[:, :], reads=[st])
                for j in range(4):
                    r0 = t0 + j * 128
                    xs = lambda k: xt_.t[:, k, j * 128:(j + 1) * 128]
                    pa = nextpp()
                    for (o0, c0, nw) in ((0, C_CKV, 256), (256, C_WI, 8), (264, C_IM, 8)):
                        for k in range(8):
                            wap, wbuf = wsl(k, c0, nw)
                            tk.op("pe", "matmul", pa.t[:, o0:o0 + nw], xs(k), wap,
                                  start=(k == 0), stop=(k == 7), reads=[wbuf, xt_], writes=[pa])
                    sm = sms[(g * 4 + j) % 4]
                    tk.op("act", "activation", out=junk.t[:, :], in_=pa.t[:, 0:256], func=AF.Square, accum_out=ssq.t[:, 0:1],
                          reads=[pa], writes=[junk, ssq])
                    tk.op("act", "activation", out=sm.t[:, 16:17], in_=ssq.t[:, :], func=AF.Sqrt, scale=1.0 / 256.0, bias=epsc.t[:, 0:1],
                          reads=[ssq, epsc], writes=[sm])
                    tk.op("dve", "reciprocal", sm.t[:, 16:17], sm.t[:, 16:17], writes=[sm])
                    tk.op("dve", "tensor_copy", sm.t[:, 0:8], pa.t[:, 256:264], reads=[pa], writes=[sm])
                    tk.op("dve", "tensor_tensor", sm.t[:, 8:12], pa.t[:, 264:268], bifs.t[:, 0:4], ALU.add,
                          reads=[pa, bifs], writes=[sm])
                    tk.op("dve", "tensor_tensor", ftmp.t[:, :], pa.t[:, 268:272], bifs.t[:, 4:8], ALU.add,
                          reads=[pa, bifs], writes=[ftmp])
                    tk.op("act", "activation", out=ftmp.t[:, :], in_=ftmp.t[:, :], func=AF.Exp, scale=-1.0, writes=[ftmp])
                    tk.op("dve", "tensor_scalar", ftmp.t[:, :], ftmp.t[:, :], 1.0, None, ALU.add, writes=[ftmp])
                    tk.op("act", "activation", out=ftmp.t[:, :], in_=ftmp.t[:, :], func=AF.Ln, writes=[ftmp])
                    tk.op("dve", "tensor_scalar", sm.t[:, 12:16], ftmp.t[:, :], -1.0, None, ALU.mult, reads=[ftmp], writes=[sm])
                    tk.dma("sp", SM[r0:r0 + 128, :], sm.t[:, :], reads=[sm])
                    pv = nextpp()
                    for k in range(8):
                        wap, wbuf = wsl(k, C_VM, 512)
                        tk.op("pe", "matmul", pv.t[:, :], xs(k), wap,
                              start=(k == 0), stop=(k == 7), reads=[wbuf, xt_], writes=[pv])
                    vm_ = vms[j % 4]
                    tk.op("act", "copy", vm_.t[:, :, 0:128], pv.t[:, :].rearrange("p (h d) -> p h d", h=4),
                          reads=[pv], writes=[vm_])
                    tk.dma("sp", VM[r0:r0 + 128, :], vm_.t[:, :, :].rearrange("p h d -> p (h d)"), reads=[vm_])
                    po = nextpp()
                    for k in range(8):
                        wap, wbuf = wsl(k, C_OM, 512)
                        tk.op("pe", "matmul", po.t[:, :], xs(k), wap,
                              start=(k == 0), stop=(k == 7), reads=[wbuf, xt_], writes=[po])
                    st = nextstg()
                    tk.op("act", "activation", out=st.t[:, :], in_=po.t[:, :], func=AF.Sigmoid, reads=[po], writes=[st])
                    tk.dma("sp", OM[r0:r0 + 128, :], st.t[:, :], reads=[st])
                    pv2 = nextpp()
                    for rc in range(2):
                        tk.op("pe", "matmul", pv2.t[:, :], cr.t[:, rc, j * 128:(j + 1) * 128], wuvg.t[:, rc, :],
                              start=(rc == 0), stop=(rc == 1), reads=[cr, wuvg], writes=[pv2])
                    va_ = vas[j % 4]
                    tk.op("dve", "tensor_scalar", va_.t[:, :, 0:64], pv2.t[:, :].rearrange("p (h d) -> p h d", h=8),
                          sm.t[:, 16:17], None, ALU.mult, reads=[pv2, sm], writes=[va_])
                    tk.dma("sp", VA[r0:r0 + 128, :], va_.t[:, :, :].rearrange("p h d -> p (h d)"), reads=[va_])
        tk.barrier()
        if stop < 3:
            tk.finish()
            return nc

        for b in range(2):
            b0 = b * S
            s34 = ExitStack()
            sbo, _ = mk(s34)
            KTs = [sbo(f"KTs{h}", [64, S], BF16) for h in range(8)]
            VAs = sbo("VAs", [128, 16, 520], BF16)
            KIs = sbo("KIs", [64, S], BF16)
            smb = sbo("smb4", [128, 16, 32], F32)
            rs = sbo("rs", [128, 16], F32)
            wua = sbo("wua", [128, 4, D], BF16)
            wum = sbo("wum", [128, 4, D], BF16)
            wo = sbo("wo", [128, 8, D], BF16)
            wrs = sbo("wrs", [128, 8, 20], F32)
            brs = sbo("brs", [128, 20], F32)
            g1 = sbo("g1", [128, D], F32)
            b1 = sbo("b1", [128, D], F32)

            def issue_s4_loads():
                for h in range(8):
                    tk.dma("sp", KTs[h].t[:], KT[h, :, b0:b0 + S], writes=[KTs[h]])
                tk.dma("sp", VAs.t[:], VA[b0:b0 + S, :].rearrange("(c p) f -> p c f", p=128), writes=[VAs])
                tk.dma("sp", KIs.t[:], KI[:, b0:b0 + S], writes=[KIs])
                tk.dma("sp", smb.t[:], SM[b0:b0 + S, :].rearrange("(c p) f -> p c f", p=128), writes=[smb])
                tk.op("dve", "tensor_scalar", rs.t[:, :], smb.t[:, :, 16], 0.125, None, ALU.mult, reads=[smb], writes=[rs])
                tk.dma("pool", wua.t[:], w_up_a.rearrange("(k p) c -> p k c", p=128), writes=[wua])
                tk.dma("pool", wum.t[:], w_up_m.rearrange("(k p) c -> p k c", p=128), writes=[wum])
                tk.dma("pool", wo.t[:], w_out.rearrange("(k p) c -> p k c", p=128), writes=[wo])
                tk.dma("sp", wrs.t[:], w_r.rearrange("(k p) c -> p k c", p=128), writes=[wrs])
                tk.dma("sp", brs.t[:], b_r, writes=[brs])
                tk.dma("sp", g1.t[:], ln1g, writes=[g1])
                tk.dma("sp", b1.t[:], ln1b, writes=[b1])

            with ExitStack() as s3:
                sb, ps = mk(s3)
                qkT = [sb(f"qkT{m}", [128, S], BF16) for m in range(8)]
                for m in range(8):
                    tk.dma("sp", qkT[m].t[:], QKM[m, :, b0:b0 + S], writes=[qkT[m]])
                vmb = sb("vmb", [128, 16, 516], BF16)
                omb = sb("omb", [128, 16, 512], BF16)
                smb3 = sb("smb3", [128, 16, 32], F32)
                tk.dma("sp", vmb.t[:], VM[b0:b0 + S, :].rearrange("(c p) f -> p c f", p=128), writes=[vmb])
                tk.dma("sp", omb.t[:], OM[b0:b0 + S, :].rearrange("(c p) f -> p c f", p=128), writes=[omb])
                tk.dma("sp", smb3.t[:], SM[b0:b0 + S, :].rearrange("(c p) f -> p c f", p=128), writes=[smb3])
                mhgs = sb("mhgs", [128, 512], F32)
                tk.dma("sp", mhgs.t[:], mhg, writes=[mhgs])
                issue_s4_loads()
                hmo = sb("hmo", [128, 16, 512], BF16)
                lf = sb("lf", [128, 64], F32)
                ii = sb("ii", [128, 64], F32)
                bc = sb("bc", [128, 64], F32)
                bt = sb("bt", [128, 64], F32)
                d1 = sb("d1", [128, 64], F32)
                d2 = sb("d2", [128, 64], F32)
                a1s = sb("a1s", [128, 64], F32)
                a2s = sb("a2s", [128, 64], F32)
                et = sb("et", [128, 64], F32)
                eb = sb("eb", [128, 64], F32)
                hbk = [ps(f"hbk{h}", [128, 512], F32) for h in range(4)]
                class _V:
                    def __init__(self, ap):
                        self.t = ap
                pST = [_V(hbk[h].t[:, 0:128]) for h in range(4)]
                pN = [_V(hbk[h].t[:, 128:257]) for h in range(4)]
                pCU = [_V(hbk[h].t[:, 257:386]) for h in range(4)]
                pKTb = ps("pKTb", [128, 512], BF16)
                pKT = [_V(pKTb.t[:, h * 128:(h + 1) * 128]) for h in range(4)]
                pg, pg2 = pST[0], pST[1]
                tk.op("dve", "tensor_copy", lf.t[:, :].rearrange("p (c h) -> p c h", h=4), smb3.t[:, :, 12:16], reads=[smb3], writes=[lf])
                tk.op("dve", "tensor_copy", ii.t[:, :].rearrange("p (c h) -> p c h", h=4), smb3.t[:, :, 8:12], reads=[smb3], writes=[ii])
                tk.op("pe", "matmul", pg.t[:, 0:64], trif.t[:, :], lf.t[:, :], start=True, stop=True, reads=[trif, lf], writes=[hbk[0]])
                tk.op("pe", "matmul", pg2.t[:, 0:64], onesf.t[:, :], lf.t[:, :], start=True, stop=True, reads=[onesf, lf], writes=[hbk[1]])
                tk.op("dve", "tensor_copy", bc.t[:, :], pg.t[:, 0:64], reads=[hbk[0]], writes=[bc])
                tk.op("dve", "tensor_copy", bt.t[:, :], pg2.t[:, 0:64], reads=[hbk[1]], writes=[bt])
                tk.op("dve", "tensor_tensor", d1.t[:, :], ii.t[:, :], bc.t[:, :], ALU.subtract, reads=[ii, bc], writes=[d1])
                tk.op("dve", "tensor_tensor", d2.t[:, :], d1.t[:, :], bt.t[:, :], ALU.add, reads=[d1, bt], writes=[d2])
                tk.op("act", "activation", out=a1s.t[:, :], in_=d1.t[:, :], func=AF.Exp, reads=[d1], writes=[a1s])
                tk.op("act", "activation", out=a2s.t[:, :], in_=d2.t[:, :], func=AF.Exp, reads=[d2], writes=[a2s])
                tk.op("act", "activation", out=et.t[:, :], in_=bc.t[:, :], func=AF.Exp, reads=[bc], writes=[et])
                tk.op("act", "activation", out=eb.t[:, :], in_=bt.t[:, :], func=AF.Exp, reads=[bt], writes=[eb])
                isd = 128.0 ** -0.5
                tk.op("dve", "tensor_scalar", a1s.t[:, :], a1s.t[:, :], isd, None, ALU.mult, writes=[a1s])
                tk.op("dve", "tensor_scalar", a2s.t[:, :], a2s.t[:, :], isd, None, ALU.mult, writes=[a2s])
                Cf = [sb(f"Cf{h}", [128, 129], F32) for h in range(4)]
                Cb = [sb(f"Cb{h}", [128, 129], BF16) for h in range(4)]
                for h in range(4):
                    tk.op("pool", "memset", Cf[h].t[:], 0.0, writes=[Cf[h]])
                STs = [sb(f"STs{i}", [128, 128], BF16) for i in range(4)]
                kss = [sb(f"kss{i}", [128, 128], BF16) for i in range(4)]
                hf = [sb(f"hf{i}", [128, 4, 128], F32) for i in range(2)]
                hj = [sb(f"hj{i}", [128, 4, 128], F32) for i in range(2)]
                hn = [sb(f"hn{i}", [128, 4, 128], F32) for i in range(2)]
                dn = [sb(f"dn{i}", [128, 4, 4], F32) for i in range(2)]
                for c in range(nchunks):
                    cs = slice(c * 128, (c + 1) * 128)
                    c4 = slice(c * 4, (c + 1) * 4)
                    hfc, hjc, hnc, dd = hf[c % 2], hj[c % 2], hn[c % 2], dn[c % 2]
                    for h in range(4):
                        tk.op("pe", "matmul", pST[h].t[:, :], qkT[4 + h].t[:, cs], qkT[h].t[:, cs], start=True, stop=True,
                              reads=[qkT[4 + h], qkT[h]], writes=[hbk[h]])
                    for h in range(4):
                        ch = c * 4 + h
                        tk.op("dve", "scalar_tensor_tensor", STs[h].t[:, :], pST[h].t[:, :], a1s.t[:, ch:ch + 1], trib.t[:, :],
                              ALU.mult, ALU.mult, reads=[hbk[h], a1s, trib], writes=[STs[h]])
                    for h in range(4):
                        vch = vmb.t[:, c, h * 129:(h + 1) * 129]
                        tk.op("pe", "matmul", pN[h].t[:, :], STs[h].t[:, :], vch, start=True, stop=(c == 0),
                              reads=[STs[h], vmb], writes=[hbk[h]])
                        if c > 0:
                            tk.op("pe", "matmul", pN[h].t[:, :], qkT[h].t[:, cs], Cb[h].t[:, :], start=False, stop=True,
                                  reads=[qkT[h], Cb[h]], writes=[hbk[h]])
                    if c < 15:
                        for h in range(4):
                            tk.op("pe", "transpose", pKT[h].t[:, :], qkT[4 + h].t[:, cs], identb.t[:, :], reads=[qkT[4 + h], identb], writes=[pKTb])
                        for h in range(4):
                            ch = c * 4 + h
                            tk.op("act", "activation", out=kss[h].t[:, :], in_=pKT[h].t[:, :], func=AF.Copy, scale=a2s.t[:, ch:ch + 1],
                                  reads=[pKTb, a2s], writes=[kss[h]])
                        for h in range(4):
                            vch = vmb.t[:, c, h * 129:(h + 1) * 129]
                            tk.op("pe", "matmul", pCU[h].t[:, :], kss[h].t[:, :], vch, start=True, stop=True,
                                  reads=[kss[h], vmb], writes=[hbk[h]])
                        for h in range(4):
                            ch = c * 4 + h
                            tk.op("dve", "scalar_tensor_tensor", Cf[h].t[:, :], Cf[h].t[:, :], eb.t[:, ch:ch + 1], pCU[h].t[:, :],
                                  ALU.mult, ALU.add, reads=[eb, hbk[h]], writes=[Cf[h]])
                        for h in range(4):
                            tk.op("act", "copy", Cb[h].t[:, :], Cf[h].t[:, :], reads=[Cf[h]], writes=[Cb[h]])
                    for h in range(4):
                        ch = c * 4 + h
                        tk.op("act", "activation", out=dd.t[:, 0, h:h + 1], in_=pN[h].t[:, 128:129], func=AF.Abs, scale=et.t[:, ch:ch + 1],
                              reads=[hbk[h], et], writes=[dd])
                    tk.op("dve", "tensor_scalar", dd.t[:, 0, :], dd.t[:, 0, :], 1.0, None, ALU.max, writes=[dd])
                    tk.op("dve", "reciprocal", dd.t[:, 1, :], dd.t[:, 0, :], writes=[dd])
                    tk.op("dve", "tensor_tensor", dd.t[:, 2, :], dd.t[:, 1, :], et.t[:, c4], ALU.mult, reads=[et], writes=[dd])
                    for h in range(4):
                        tk.op("act", "activation", out=hfc.t[:, h, :], in_=pN[h].t[:, 0:128], func=AF.Copy, scale=dd.t[:, 2, h:h + 1],
                              reads=[hbk[h], dd], writes=[hfc])
                    tk.op("act", "activation", out=hjc.t[:, :, :], in_=hfc.t[:, :, :], func=AF.Square, reads=[hfc], writes=[hjc])
                    tk.op("dve", "tensor_reduce", dd.t[:, 3, :], hjc.t[:, :, :], AX.X, ALU.add, reads=[hjc], writes=[dd])
                    tk.op("act", "activation", out=dd.t[:, 3, :], in_=dd.t[:, 3, :], func=AF.Sqrt, scale=1.0 / 128.0, bias=epsc.t[:, 0:1],
                          reads=[epsc], writes=[dd])
                    tk.op("dve", "reciprocal", dd.t[:, 3, :], dd.t[:, 3, :], writes=[dd])
                    tk.op("dve", "tensor_tensor", hnc.t[:, :, :], hfc.t[:, :, :], mhgs.t[:, :].rearrange("p (h d) -> p h d", h=4), ALU.mult,
                          reads=[hfc, mhgs], writes=[hnc])
                    tk.op("dve", "tensor_tensor", hnc.t[:, :, :], hnc.t[:, :, :], dd.t[:, 3, :].unsqueeze(2).broadcast_to([128, 4, 128]), ALU.mult,
                          reads=[dd], writes=[hnc])
                    tk.op("pool", "tensor_tensor", hmo.t[:, c, :], hnc.t[:, :, :].rearrange("p h d -> p (h d)"), omb.t[:, c, :],
                          ALU.mult, reads=[hnc, omb], writes=[hmo])
                tk.dma("sp", HM[b0:b0 + S, :].rearrange("(c p) f -> p c f", p=128), hmo.t[:], reads=[hmo])
            tk.barrier()
            if stop < 4:
                tk.finish()
                return nc

            with ExitStack() as s4:
                sb, ps = mk(s4)
                QIt = [sb(f"QIt{i}", [64, 8, 128], BF16) for i in range(2)]
                QAt = [sb(f"QAt{i}", [64, 8, 128], BF16) for i in range(2)]
                GTt = [sb(f"GTt{i}", [128, 16, 128], BF16) for i in range(3)]
                HMt = [sb(f"HMt{i}", [128, 512], BF16) for i in range(3)]
                xt = [sb(f"xt{i}", [128, D], F32) for i in range(3)]
                score = sb("score", [128, S], F32)
                work = sb("work", [128, S], BF16)
                rstg = [sb(f"rstg{i}", [128, 512], F32) for i in range(2)]
                m8 = sb("m8", [128, 8], F32)
                mask = sb("mask", [128, S], BF16)
                mTs = [sb(f"mT{i}", [128, 16, 128], BF16) for i in range(2)]
                blo = sb("blo", [128, 1], F32)
                bw0 = sb("bw0", [128, 1], F32)
                bmid = sb("bmid", [128, 1], F32)
                bwt = sb("bwt", [128, NBIS + 1], F32)
                bcnt = sb("bcnt", [128, 1], F32)
                bsw = sb("bsw", [128, 1], F32)
                ME = [sb(f"ME{i}", [128, 8, 128], BF16) for i in range(2)]
                Pb = [[sb(f"Pb{i}{hh}", [128, 512], BF16) for hh in range(2)] for i in range(2)]
                oa = sb("oa", [128, 512], BF16)
                rden = sb("rden", [128, 8], F32)
                osbs = [sb(f"osb{i}", [128, 2, 260], F32) for i in range(2)]
                oaT = sb("oaT", [128, 4, 128], BF16)
                hmT = sb("hmT", [128, 4, 128], BF16)
                t1 = sb("t1", [128, D], F32)
                t2 = sb("t2", [128, D], F32)
                yT = sb("yT", [128, 8, 128], BF16)
                z = sb("z", [128, D], F32)
                x1 = sb("x1", [128, D], F32)
                stats = sb("stats", [128, 12], F32)
                mv = sb("mv", [128, 4], F32)
                x1Tf = sb("x1Tf", [128, 8, 128], F32)
                x1Tb = sb("x1Tb", [128, 8, 128], BF16)
                rt = sb("rt", [128, 64], F32)
                cmbt = sb("cmbt", [128, 16], F32)
                pS = [ps(f"pS{i}", [128, 512], F32) for i in range(2)]
                pTm = ps("pTm", [128, 1024], BF16)
                pL = [ps(f"pL{i}", [128, 512], F32) for i in range(2)]
                pO = [ps(f"pO{i}", [128, 512], F32) for i in range(2)]
                pR = ps("pR", [128, 512], F32)

                def A1g(i):
                    q0 = b0 + i * 128
                    N = 128 * (i + 1)
                    qi, qa, gt, hm, xti = QIt[i % 2], QAt[i % 2], GTt[i % 3], HMt[i % 3], xt[i % 3]
                    tk.dma("sp", qa.t[:], QA[:, :, q0:q0 + 128].rearrange("h d t -> d h t"), writes=[qa])
                    tk.dma("sp", gt.t[:], GT[:, :, q0:q0 + 128].rearrange("m p t -> p m t"), writes=[gt])
                    tk.dma("sp", hm.t[:], HM[q0:q0 + 128, :], writes=[hm])
                    tk.dma("sp", xti.t[:], x[q0:q0 + 128, :], writes=[xti])
                    if i >= 2:
                        nch = (N + 511) // 512
                        n = 0
                        for h in range(8):
                            for cch in range(nch):
                                c0 = cch * 512
                                cwid = min(512, N - c0)
                                p = pS[n % 2]
                                rg = rstg[n % 2]
                                n += 1
                                tk.op("pe", "matmul", p.t[:, :cwid], qi.t[:, h, :], KIs.t[:, c0:c0 + cwid], start=True, stop=True,
                                      reads=[qi, KIs], writes=[p])
                                tk.op("act", "activation", out=rg.t[:, :cwid], in_=p.t[:, :cwid], func=AF.Relu, reads=[p], writes=[rg])
                                if h == 0:
                                    tk.op("dve", "tensor_scalar", score.t[:, c0:c0 + cwid], rg.t[:, :cwid], smb.t[:, i, 0:1], None,
                                          ALU.mult, reads=[rg, smb], writes=[score])
                                else:
                                    tk.op("dve", "scalar_tensor_tensor", score.t[:, c0:c0 + cwid], rg.t[:, :cwid], smb.t[:, i, h:h + 1],
                                          score.t[:, c0:c0 + cwid], ALU.mult, ALU.add, reads=[rg, smb], writes=[score])
                                yield
                        yield "scores_done"
                        tk.op("dve", "tensor_reduce", blo.t[:, :], score.t[:, :N], AX.X, ALU.min, reads=[score], writes=[blo])
                        tk.op("dve", "tensor_tensor", score.t[:, N - 128:N], score.t[:, N - 128:N], trineg.t[:, :], ALU.add,
                              reads=[trineg], writes=[score])
                        tk.op("dve", "tensor_reduce", bw0.t[:, :], score.t[:, :N], AX.X, ALU.max, reads=[score], writes=[bw0])
                        tk.op("dve", "tensor_tensor", bw0.t[:, :], bw0.t[:, :], blo.t[:, :], ALU.subtract, reads=[blo], writes=[bw0])
                        tk.op("dve", "tensor_scalar", bwt.t[:, :], ckc.t[:, :], bw0.t[:, 0:1], None, ALU.mult, reads=[ckc, bw0], writes=[bwt])
                        tk.op("dve", "tensor_tensor", bmid.t[:, :], blo.t[:, :], bwt.t[:, 0:1], ALU.add, reads=[blo, bwt], writes=[bmid])
                        for r in range(NBIS):
                            tk.op("dve", "tensor_scalar", work.t[:, :N], score.t[:, :N], bmid.t[:, 0:1], 0.0, ALU.is_ge, ALU.add,
                                  accum_out=bcnt.t[:, 0:1], reads=[score, bmid], writes=[work, bcnt])
                            tk.op("dve", "tensor_scalar", bsw.t[:, :], bcnt.t[:, :], 255.5, 0.5, ALU.is_ge, ALU.subtract, reads=[bcnt], writes=[bsw])
                            tk.op("dve", "scalar_tensor_tensor", bmid.t[:, :], bsw.t[:, :], bwt.t[:, r:r + 1], bmid.t[:, :], ALU.mult, ALU.add,
                                  reads=[bsw, bwt], writes=[bmid])
                            yield
                        tk.op("dve", "tensor_tensor", blo.t[:, :], bmid.t[:, :], bwt.t[:, NBIS:NBIS + 1], ALU.subtract, reads=[bmid, bwt], writes=[blo])
                        tk.op("dve", "tensor_scalar", mask.t[:, :N], score.t[:, :N], blo.t[:, 0:1], None, ALU.is_ge,
                              reads=[score, blo], writes=[mask])
                    return
                    yield

                def load_qi(i):
                    if 2 <= i < ntile:
                        q0_ = b0 + i * 128
                        tk.dma("sp", QIt[i % 2].t[:], QI[:, :, q0_:q0_ + 128].rearrange("h d t -> d h t"), writes=[QIt[i % 2]])

                def A2(i):
                    mT = mTs[i % 2]
                    if i >= 2:
                        for j0 in range(0, i + 1, 8):
                            nb = min(8, i + 1 - j0)
                            for jj in range(nb):
                                j = j0 + jj
                                tk.op("pe", "transpose", pTm.t[:, jj * 128:(jj + 1) * 128], mask.t[:, j * 128:(j + 1) * 128], identb.t[:, :],
                                      reads=[mask, identb], writes=[pTm])
                            tk.op("act", "copy", mT.t[:, j0:j0 + nb, :], pTm.t[:, :nb * 128].rearrange("p (j t) -> p j t", t=128),
                                  reads=[pTm], writes=[mT])
                    else:
                        for j in range(i):
                            tk.op("pool", "tensor_copy", mT.t[:, j, :], onesb.t[:, :], reads=[onesb], writes=[mT])
                        tk.op("pool", "tensor_copy", mT.t[:, i, :], trib.t[:, :], reads=[trib], writes=[mT])

                def Bg(i):
                    q0 = b0 + i * 128
                    N = 128 * (i + 1)
                    qi, qa, gt, hm, xti = QIt[i % 2], QAt[i % 2], GTt[i % 3], HMt[i % 3], xt[i % 3]
                    mT = mTs[i % 2]
                    tk.op("pool", "tensor_tensor", ME[0].t[:, :, :], erel.t[:, 0, :, :], mT.t[:, i, :].unsqueeze(1).broadcast_to([128, 8, 128]),
                          ALU.mult, reads=[erel, mT], writes=[ME[0]])
                    if i > 0:
                        tk.op("pool", "tensor_tensor", ME[1].t[:, :, :], erel.t[:, 1, :, :], mT.t[:, i - 1, :].unsqueeze(1).broadcast_to([128, 8, 128]),
                              ALU.mult, reads=[erel, mT], writes=[ME[1]])
                    hps = [(j, hh) for j in range(i + 1) for hh in range(2)]

                    def front(j, hh):
                        js = slice(j * 128, (j + 1) * 128)
                        pb = Pb[j % 2]
                        for h4 in range(4):
                            h = hh * 4 + h4
                            tk.op("pe", "matmul", pL[hh].t[:, h4 * 128:(h4 + 1) * 128], KTs[h].t[:, js], qa.t[:, h, :],
                                  start=True, stop=True, reads=[KTs[h], qa], writes=[pL[hh]])
                        tk.op("act", "activation", out=pb[hh].t[:, :], in_=pL[hh].t[:, :], func=AF.Exp, scale=rs.t[:, j:j + 1],
                              reads=[pL[hh], rs], writes=[pb[hh]])
                        pv = pb[hh].t[:, :].rearrange("p (h t) -> p h t", h=4)
                        if j == i:
                            mk_ap, mk_b = ME[0].t[:, hh * 4:(hh + 1) * 4, :], ME[0]
                        elif j == i - 1:
                            mk_ap, mk_b = ME[1].t[:, hh * 4:(hh + 1) * 4, :], ME[1]
                        else:
                            mk_ap, mk_b = mT.t[:, j, :].unsqueeze(1).broadcast_to([128, 4, 128]), mT
                        tk.op("pool", "tensor_tensor", pv, pv, mk_ap, ALU.mult, reads=[mk_b], writes=[pb[hh]])

                    def back(j, hh):
                        pb = Pb[j % 2]
                        for h4 in range(4):
                            h = hh * 4 + h4
                            tk.op("pe", "matmul", pO[hh].t[:, h4 * 65:(h4 + 1) * 65], pb[hh].t[:, h4 * 128:(h4 + 1) * 128],
                                  VAs.t[:, j, h * 65:(h + 1) * 65], start=(j == 0 and h4 == 0), stop=(j == i), skip_group_check=True,
                                  reads=[pb[hh], VAs], writes=[pO[hh]])

                    front(*hps[0])
                    front(*hps[1])
                    for kk in range(len(hps)):
                        if kk + 2 < len(hps):
                            front(*hps[kk + 2])
                        back(*hps[kk])
                        yield
                    osb = osbs[i % 2]
                    for hh in range(2):
                        tk.op("act", "copy", osb.t[:, hh, :], pO[hh].t[:, 0:260], reads=[pO[hh]], writes=[osb])

                def tail(i):
                    q0 = b0 + i * 128
                    gt, hm, xti = GTt[i % 3], HMt[i % 3], xt[i % 3]
                    osb = osbs[i % 2]
                    for hh in range(2):
                        ov = osb.t[:, hh, :].rearrange("p (h d) -> p h d", d=65)
                        tk.op("dve", "reciprocal", rden.t[:, hh * 4:(hh + 1) * 4], ov[:, :, 64], reads=[osb], writes=[rden])
                        tk.op("dve", "tensor_tensor", oa.t[:, hh * 256:(hh + 1) * 256].rearrange("p (h d) -> p h d", d=64), ov[:, :, 0:64],
                              rden.t[:, hh * 4:(hh + 1) * 4].unsqueeze(2).broadcast_to([128, 4, 64]), ALU.mult, reads=[osb, rden], writes=[oa])
                    for k in range(4):
                        tk.op("pe", "transpose", pTm.t[:, k * 128:(k + 1) * 128], oa.t[:, k * 128:(k + 1) * 128], identb.t[:, :],
                              reads=[oa, identb], writes=[pTm])
                    for k in range(4):
                        tk.op("pe", "transpose", pTm.t[:, (4 + k) * 128:(5 + k) * 128], hm.t[:, k * 128:(k + 1) * 128], identb.t[:, :],
                              reads=[hm, identb], writes=[pTm])
                    tk.op("act", "copy", oaT.t[:, :, :], pTm.t[:, 0:512].rearrange("p (k t) -> p k t", t=128), reads=[pTm], writes=[oaT])
                    tk.op("act", "copy", hmT.t[:, :, :], pTm.t[:, 512:1024].rearrange("p (k t) -> p k t", t=128), reads=[pTm], writes=[hmT])

                    for hh in range(2):
                        hs = slice(hh * 512, (hh + 1) * 512)
                        for m4 in range(4):
                            m = hh * 4 + m4
                            for k in range(4):
                                tk.op("pe", "matmul", pR.t[:, m4 * 128:(m4 + 1) * 128], wua.t[:, k, m * 128:(m + 1) * 128], oaT.t[:, k, :],
                                      start=(k == 0), stop=(k == 3), reads=[wua, oaT], writes=[pR])
                        yield
                        tk.op("dve", "tensor_tensor", t1.t[:, hs], pR.t[:, :], gt.t[:, hh * 4:(hh + 1) * 4, :].rearrange("p m t -> p (m t)"),
                              ALU.mult, reads=[pR, gt], writes=[t1])
                        for m4 in range(4):
                            m = hh * 4 + m4
                            for k in range(4):
                                tk.op("pe", "matmul", pR.t[:, m4 * 128:(m4 + 1) * 128], wum.t[:, k, m * 128:(m + 1) * 128], hmT.t[:, k, :],
                                      start=(k == 0), stop=(k == 3), reads=[wum, hmT], writes=[pR])
                        yield
                        tk.op("dve", "tensor_tensor", t2.t[:, hs], pR.t[:, :], gt.t[:, 8 + hh * 4:8 + (hh + 1) * 4, :].rearrange("p m t -> p (m t)"),
                              ALU.mult, reads=[pR, gt], writes=[t2])
                        tk.op("pool", "tensor_tensor", yT.t[:, hh * 4:(hh + 1) * 4, :].rearrange("p m t -> p (m t)"), t1.t[:, hs], t2.t[:, hs],
                              ALU.add, reads=[t1, t2], writes=[yT])
                    for nn in range(2):
                        for k in range(8):
                            tk.op("pe", "matmul", pR.t[:, :], yT.t[:, k, :], wo.t[:, k, nn * 512:(nn + 1) * 512],
                                  start=(k == 0), stop=(k == 7), reads=[yT, wo], writes=[pR])
                        yield
                        ns = slice(nn * 512, (nn + 1) * 512)
                        tk.op("dve", "scalar_tensor_tensor", z.t[:, ns], xti.t[:, ns], ALPHA, pR.t[:, :], ALU.mult, ALU.add,
                              reads=[xti, pR], writes=[z])
                    tk.op("act", "activation", out=t1.t[:, :], in_=z.t[:, :], func=AF.Identity, accum_out=mv.t[:, 0:1], reads=[z], writes=[t1, mv])
                    tk.op("act", "activation", out=t1.t[:, :], in_=z.t[:, :], func=AF.Square, accum_out=mv.t[:, 1:2], reads=[z], writes=[t1, mv])
                    yield
                    tk.op("dve", "tensor_scalar", mv.t[:, 0:1], mv.t[:, 0:1], 1.0 / D, None, ALU.mult, writes=[mv])
                    tk.op("dve", "tensor_tensor", mv.t[:, 3:4], mv.t[:, 0:1], mv.t[:, 0:1], ALU.mult, writes=[mv])
                    tk.op("dve", "scalar_tensor_tensor", mv.t[:, 1:2], mv.t[:, 1:2], 1.0 / D, mv.t[:, 3:4], ALU.mult, ALU.subtract, writes=[mv])
                    tk.op("act", "activation", out=mv.t[:, 2:3], in_=mv.t[:, 1:2], func=AF.Sqrt, bias=epsc.t[:, 0:1], reads=[epsc], writes=[mv])
                    yield
                    tk.op("dve", "reciprocal", mv.t[:, 2:3], mv.t[:, 2:3], writes=[mv])
                    tk.op("dve", "tensor_scalar", x1.t[:, :], z.t[:, :], mv.t[:, 0:1], mv.t[:, 2:3], ALU.subtract, ALU.mult, reads=[z, mv], writes=[x1])
                    tk.op("pool", "tensor_tensor", x1.t[:, :], x1.t[:, :], g1.t[:, :], ALU.mult, reads=[g1], writes=[x1])
                    tk.op("pool", "tensor_tensor", x1.t[:, :], x1.t[:, :], b1.t[:, :], ALU.add, reads=[b1], writes=[x1])
                    tk.dma("sp", X1[q0:q0 + 128, :], x1.t[:, :], reads=[x1])
                    for hh in range(2):
                        for k4 in range(4):
                            k = hh * 4 + k4
                            tk.op("pe", "matmul", pR.t[:, k4 * 128:(k4 + 1) * 128], x1.t[:, k * 128:(k + 1) * 128], identf.t[:, :],
                                  start=True, stop=True, reads=[x1, identf], writes=[pR])
                        tk.op("act", "copy", x1Tf.t[:, hh * 4:(hh + 1) * 4, :].rearrange("p k t -> p (k t)"), pR.t[:, :], reads=[pR], writes=[x1Tf])
                        tk.op("act", "copy", x1Tb.t[:, hh * 4:(hh + 1) * 4, :].rearrange("p k t -> p (k t)"), pR.t[:, :], reads=[pR], writes=[x1Tb])
                        yield
                    tk.dma("sp", X1T[:, :, q0:q0 + 128].rearrange("k p t -> p k t"), x1Tb.t[:, :, :], reads=[x1Tb])
                    for k in range(8):
                        tk.op("pe", "matmul", pR.t[:, 0:20], x1Tf.t[:, k, :], wrs.t[:, k, :], start=(k == 0), stop=(k == 7),
                              reads=[x1Tf, wrs], writes=[pR])
                    yield
                    L = rt.t
                    W = dict(reads=[], writes=[rt])
                    tk.op("dve", "tensor_tensor", L[:, 0:20], pR.t[:, 0:20], brs.t[:, :], ALU.add, reads=[pR, brs], writes=[rt])
                    tk.op("dve", "reduce_max", L[:, 20:21], L[:, 0:4], AX.X, **W)
                    tk.op("dve", "tensor_scalar", L[:, 24:28], L[:, 0:4], L[:, 20:21], None, ALU.is_ge, **W)
                    tk.op("dve", "tensor_scalar", L[:, 28:32], L[:, 0:4], L[:, 20:21], None, ALU.subtract, **W)
                    tk.op("act", "activation", out=L[:, 28:32], in_=L[:, 28:32], func=AF.Exp, accum_out=L[:, 21:22], **W)
                    tk.op("dve", "tensor_tensor", L[:, 32:48].rearrange("p (g e) -> p g e", e=4), L[:, 4:20].rearrange("p (g e) -> p g e", e=4),
                          L[:, 24:28].unsqueeze(2).broadcast_to([128, 4, 4]), ALU.mult, **W)
                    tk.op("dve", "tensor_reduce", L[:, 48:52], L[:, 32:48].rearrange("p (g e) -> p e g", e=4), AX.X, ALU.add, **W)
                    tk.op("dve", "reduce_max", L[:, 52:53], L[:, 48:52], AX.X, **W)
                    tk.op("dve", "tensor_scalar", L[:, 56:60], L[:, 48:52], L[:, 52:53], None, ALU.is_ge, **W)
                    tk.op("dve", "scalar_tensor_tensor", L[:, 60:64], L[:, 56:60], NEG, L[:, 48:52], ALU.mult, ALU.add, **W)
                    tk.op("dve", "reduce_max", L[:, 53:54], L[:, 60:64], AX.X, **W)
                    tk.op("dve", "tensor_scalar", L[:, 60:64], L[:, 60:64], L[:, 53:54], None, ALU.is_ge, **W)
                    tk.op("dve", "tensor_tensor", L[:, 54:55], L[:, 53:54], L[:, 52:53], ALU.subtract, **W)
                    tk.op("act", "activation", out=L[:, 55:56], in_=L[:, 54:55], func=AF.Sigmoid, scale=-1.0, **W)
                    yield
                    tk.op("dve", "reciprocal", L[:, 22:23], L[:, 21:22], **W)
                    tk.op("dve", "tensor_tensor", L[:, 55:56], L[:, 55:56], L[:, 22:23], ALU.mult, **W)
                    tk.op("dve", "tensor_tensor", L[:, 54:55], L[:, 22:23], L[:, 55:56], ALU.subtract, **W)
                    tk.op("dve", "tensor_scalar", L[:, 56:60], L[:, 56:60], L[:, 55:56], None, ALU.mult, **W)
                    tk.op("dve", "scalar_tensor_tensor", L[:, 56:60], L[:, 60:64], L[:, 54:55], L[:, 56:60], ALU.mult, ALU.add, **W)
                    tk.op("dve", "tensor_tensor", cmbt.t[:, :].rearrange("p (g e) -> p g e", e=4),
                          L[:, 24:28].unsqueeze(2).broadcast_to([128, 4, 4]), L[:, 56:60].unsqueeze(1).broadcast_to([128, 4, 4]),
                          ALU.mult, reads=[rt], writes=[cmbt])
                    tk.dma("sp", CMB[q0:q0 + 128, :], cmbt.t[:, :], reads=[cmbt])

                def adv(g, n=1):
                    if g is None:
                        return
                    for _ in range(n):
                        if next(g, "done") == "done":
                            return

                def drain(g):
                    if g is not None:
                        for _ in g:
                            pass

                def sched(a1, others, nscore):
                    others = [o for o in others if o is not None]
                    if a1 is None:
                        for g, _ in reversed(others):
                            drain(g)
                        return
                    total = SCW * nscore + NBIS
                    credit = [0.0] * len(others)
                    done = [False] * len(others)
                    wgt = SCW
                    while True:
                        v = next(a1, "done")
                        if v == "done":
                            break
                        if v == "scores_done":
                            wgt = 1.0
                            continue
                        for oi, (g, nsteps) in enumerate(others):
                            if done[oi]:
                                continue
                            credit[oi] += (nsteps + 1) * wgt / total
                            while credit[oi] >= 1.0 and not done[oi]:
                                credit[oi] -= 1.0
                                if next(g, "done") == "done":
                                    done[oi] = True
                    for g, _ in others:
                        drain(g)

                load_qi(2)
                drain(A1g(0))
                A2(0)
                for i in range(ntile):
                    if i >= 1:
                        load_qi(i + 2)
                    a1 = A1g(i + 1) if i + 1 < ntile else None
                    if a1 is not None and i + 1 < 2:
                        drain(a1)
                        a1 = None
                    sched(a1, [(Bg(i), 2 * (i + 1)), (tail(i - 1), 13) if i >= 1 else None], 8 * ((128 * (i + 2) + 511) // 512))
                    if i + 1 < ntile:
                        A2(i + 1)
                drain(tail(ntile - 1))
            s34.close()
            tk.barrier()
            if stop < 5:
                tk.finish()
                return nc

        with ExitStack() as s5:
            sb, ps = mk(s5)
            x1T = [sb(f"x1T{k}", [128, S], BF16) for k in range(8)]
            x1Tg = [[Buf(x1T[k].t[:, g * 512:(g + 1) * 512]) for g in range(4)] for k in range(8)]
            acc = [sb(f"acc{t_}", [128, D], F32) for t_ in range(16)]
            cmbs = [sb(f"cmb{i}", [128, 16, 16], F32) for i in range(2)]
            eps2 = sb("eps2", [128, 1], F32)
            tk.op("dve", "memset", eps2.t[:], LN_EPS / (ALPHA * ALPHA), writes=[eps2])
            g2 = sb("g2", [128, D], F32)
            b2 = sb("b2", [128, D], F32)
            wg = [sb(f"wg{i}", [128, 8, 512], BF16) for i in range(2)]
            wu = [sb(f"wu{i}", [128, 8, 512], BF16) for i in range(2)]
            wd = [sb(f"wd{i}", [128, 4, D], BF16) for i in range(2)]
            hT = [sb(f"hT{i}", [128, 4, 512], BF16) for i in range(2)]
            sg = [sb(f"sg{i}", [128, 512], F32) for i in range(2)]
            ot = [sb(f"ot{i}", [128, D], F32) for i in range(2)]
            mv = sb("mv5", [128, 4], F32)
            pG = [ps(f"pG{i}", [128, 512], F32) for i in range(2)]
            pU = [ps(f"pU{i}", [128, 512], F32) for i in range(2)]
            pD = [ps(f"pD{i}", [128, 512], F32) for i in range(4)]

            def load_x1T(b, grp):
                c0 = b * S + grp * 512
                for k in range(8):
                    tk.dma("sp", x1Tg[k][grp].t, X1T[k, :, c0:c0 + 512], writes=[x1Tg[k][grp]])

            def load_acc(b, t_):
                r0 = b * S + t_ * 128
                tk.dma("sp", acc[t_].t[:], X1[r0:r0 + 128, :], writes=[acc[t_]])

            def load_cmb(b):
                cm = cmbs[b]
                tk.dma("sp", cm.t[:], CMB[b * S:(b + 1) * S, :].rearrange("(c p) e -> p c e", p=128), writes=[cm])
                tk.op("dve", "tensor_scalar", cm.t[:, :, :], cm.t[:, :, :], 1.0 / ALPHA, None, ALU.mult, writes=[cm])

            def loadw(ge):
                e, i2 = ge % 16, ge % 2
                tk.dma("pool", wg[i2].t[:], w_gate[e].rearrange("(k p) f -> p k f", p=128), writes=[wg[i2]])
                tk.dma("pool", wu[i2].t[:], w_up[e].rearrange("(k p) f -> p k f", p=128), writes=[wu[i2]])
                tk.dma("pool", wd[i2].t[:], w_down[e].rearrange("(k p) f -> p k f", p=128), writes=[wd[i2]])

            def ln2(b, tt):
                a = acc[tt]
                o = ot[tt % 2]
                r0 = b * S + tt * 128
                tk.op("act", "activation", out=o.t[:, :], in_=a.t[:, :], func=AF.Identity, accum_out=mv.t[:, 0:1], reads=[a], writes=[o, mv])
                tk.op("act", "activation", out=o.t[:, :], in_=a.t[:, :], func=AF.Square, accum_out=mv.t[:, 1:2], reads=[a], writes=[o, mv])
                tk.op("dve", "tensor_scalar", mv.t[:, 0:1], mv.t[:, 0:1], 1.0 / D, None, ALU.mult, writes=[mv])
                tk.op("dve", "tensor_tensor", mv.t[:, 3:4], mv.t[:, 0:1], mv.t[:, 0:1], ALU.mult, writes=[mv])
                tk.op("dve", "scalar_tensor_tensor", mv.t[:, 1:2], mv.t[:, 1:2], 1.0 / D, mv.t[:, 3:4], ALU.mult, ALU.subtract, writes=[mv])
                tk.op("act", "activation", out=mv.t[:, 2:3], in_=mv.t[:, 1:2], func=AF.Sqrt, bias=eps2.t[:, 0:1], reads=[eps2], writes=[mv])
                tk.op("dve", "reciprocal", mv.t[:, 2:3], mv.t[:, 2:3], writes=[mv])
                tk.op("dve", "tensor_scalar", o.t[:, :], a.t[:, :], mv.t[:, 0:1], mv.t[:, 2:3], ALU.subtract, ALU.mult, reads=[a, mv], writes=[o])
                tk.op("pool", "tensor_tensor", o.t[:, :], o.t[:, :], g2.t[:, :], ALU.mult, reads=[g2], writes=[o])
                tk.op("pool", "tensor_tensor", o.t[:, :], o.t[:, :], b2.t[:, :], ALU.add, reads=[b2], writes=[o])
                tk.dma("sp", out[r0:r0 + 128, :], o.t[:, :], reads=[o])

            loadw(0)
            for grp in range(4):
                load_x1T(0, grp)
            load_cmb(0)
            for t_ in range(16):
                load_acc(0, t_)
            tk.dma("sp", g2.t[:], ln2g, writes=[g2])
            tk.dma("sp", b2.t[:], ln2b, writes=[b2])
            n = 0
            nd = 0
            for b in range(2):
                cmb = cmbs[b]
                for e in range(16):
                    ge = b * 16 + e
                    last = (e == 15)
                    if ge + 1 < 32:
                        loadw(ge + 1)
                    if last and b == 0:
                        load_cmb(1)
                    i2 = ge % 2
                    for grp in range(4):
                        gs = slice(grp * 512, (grp + 1) * 512)
                        hb = hT[grp % 2]
                        for f in range(4):
                            pg_, pu_, sg_ = pG[n % 2], pU[n % 2], sg[n % 2]
                            n += 1
                            for k in range(8):
                                tk.op("pe", "matmul", pg_.t[:, :], wg[i2].t[:, k, f * 128:(f + 1) * 128], x1T[k].t[:, gs],
                                      start=(k == 0), stop=(k == 7), reads=[wg[i2], x1Tg[k][grp]], writes=[pg_])
                            for k in range(8):
                                tk.op("pe", "matmul", pu_.t[:, :], wu[i2].t[:, k, f * 128:(f + 1) * 128], x1T[k].t[:, gs],
                                      start=(k == 0), stop=(k == 7), reads=[wu[i2], x1Tg[k][grp]], writes=[pu_])
                            tk.op("act", "activation", out=sg_.t[:, :], in_=pg_.t[:, :], func=AF.Silu, reads=[pg_], writes=[sg_])
                            tk.op("dve", "tensor_tensor", hb.t[:, f, :], sg_.t[:, :], pu_.t[:, :], ALU.mult, reads=[sg_, pu_], writes=[hb])
                        if last and b == 0:
                            load_x1T(1, grp)
                        for j in range(4):
                            tt = grp * 4 + j
                            for nn in range(2):
                                pd_ = pD[nd % 4]
                                nd += 1
                                for f in range(4):
                                    tk.op("pe", "matmul", pd_.t[:, :], hb.t[:, f, j * 128:(j + 1) * 128], wd[i2].t[:, f, nn * 512:(nn + 1) * 512],
                                          start=(f == 0), stop=(f == 3), reads=[hb, wd[i2]], writes=[pd_])
                                ns = slice(nn * 512, (nn + 1) * 512)
                                tk.op("dve", "scalar_tensor_tensor", acc[tt].t[:, ns], pd_.t[:, :], cmb.t[:, tt, e:e + 1], acc[tt].t[:, ns],
                                      ALU.mult, ALU.add, reads=[pd_, cmb], writes=[acc[tt]])
                            if last:
                                ln2(b, tt)
                                if b == 0:
                                    load_acc(1, tt)
        tk.barrier()
        tk.finish()
    return nc


def _t5_bucket_np(d):
    d = np.maximum(d, 0)
    ratio = np.log(np.maximum(d, 1).astype(np.float32) / np.float32(16)) / np.float32(math.log(128 / 16))
    large = np.minimum(16 + (ratio * 16).astype(np.int32), 31)
    return np.where(d < 16, d, large)


def _consts():
    idx = np.arange(128)
    ident = np.eye(128, dtype=np.float32)
    tri = (idx[:, None] <= idx[None, :]).astype(np.float32)
    trineg = np.where(idx[None, :] <= idx[:, None], 0.0, NEG).astype(np.float32)
    ones = np.ones((128, 128), np.float32)
    oh = np.zeros((32, 2, 128, 128), np.float32)
    for blk in range(2):
        dd = idx[:, None] - idx[None, :] + 128 * blk
        bk = _t5_bucket_np(dd)
        for bb in range(32):
            oh[bb, blk] = (bk == bb)
    return ident, tri, trineg, ones, oh


def kernel(x, w_in, conv_w, conv_b, kv_norm_g, w_uk, w_uv, rel_bias, b_i, b_f, mh_norm_g, w_up_a, w_up_m, w_out,
           ln1_g, ln1_b, w_grp, b_grp, w_rt, b_rt, w_gate, w_up, w_down, ln2_g, ln2_b):
    f = lambda a: np.ascontiguousarray(np.asarray(a, dtype=np.float32))
    x = f(x)
    rep = lambda v, n: f(np.broadcast_to(np.asarray(v, np.float32).reshape(1, n), (128, n)))
    ident, tri, trineg, ones, oh = _consts()
    rel_bias = f(rel_bias)
    shared = {
        "w_in": f(w_in[0]),
        "conv_w": f(np.asarray(conv_w[0]).T.reshape(8, 128, 4).transpose(1, 0, 2)),
        "conv_b": f(np.asarray(conv_b[0]).reshape(8, 128, 1).transpose(1, 0, 2)),
        "kvg": f(np.asarray(kv_norm_g[0]).reshape(2, 128, 1).transpose(1, 0, 2)),
        "w_uk": f(np.asarray(w_uk[0]).reshape(256, 512)),
        "w_uv": f(np.asarray(w_uv[0]).reshape(256, 512)),
        "relb": rel_bias,
        "relb31": f(np.broadcast_to(rel_bias[31:32, :], (32, 8))),
        "oh": oh,
        "bif": rep(np.concatenate([np.asarray(b_i[0]), np.asarray(b_f[0])]), 8),
        "mhg": rep(np.asarray(mh_norm_g[0]).reshape(-1), 512),
        "w_up_a": f(w_up_a[0]), "w_up_m": f(w_up_m[0]), "w_out": f(w_out[0]),
        "ln1g": rep(ln1_g[0], D), "ln1b": rep(ln1_b[0], D), "ln2g": rep(ln2_g[0], D), "ln2b": rep(ln2_b[0], D),
        "w_r": f(np.concatenate([np.asarray(w_grp[0]), np.asarray(w_rt[0])], axis=1)),
        "b_r": rep(np.concatenate([np.asarray(b_grp[0]), np.asarray(b_rt[0])]), 20),
        "w_gate": f(w_gate[0]), "w_up": f(w_up[0]), "w_down": f(w_down[0]),
        "c_ident": ident, "c_tri": tri, "c_trineg": trineg, "c_ones": ones,
        "c_ck": np.ascontiguousarray(np.broadcast_to((0.5 ** np.arange(1, NBIS + 2, dtype=np.float64)).astype(np.float32)[None, :], (128, NBIS + 1))),
    }
    if _DBG.get("on"):
        return shared, x
    nc = build_nc()
    in_maps = []
    for c in range(NCORES):
        m = dict(shared)
        m["x"] = np.ascontiguousarray(x[2 * c:2 * c + 2].reshape(T, D))
        in_maps.append(m)
    res = run_bass_kernel_spmd(nc, in_maps, core_ids=list(range(NCORES)))
    outs = [np.asarray(r["out"], dtype=np.float32).reshape(2, S, D) for r in res.results]
    return np.concatenate(outs, axis=0)
```
